# Optimizing a Trainium2 kernel written in Bass

```python
import math
import functools
import jax
import jax.numpy as jnp
from jax import lax
import numpy as np

D_MODEL = 1024
BATCH = 32
SEQ = 2048
DEPTH = 1
DEC_BATCH = 128
DEC_SEQ = 4
PAST_LEN = 8192
PAGE_SIZE = 128

D_ATTN = D_MODEL // 2
D_SSM = D_MODEL - D_ATTN
HEAD_DIM = 64
N_HEADS = D_ATTN // HEAD_DIM
DILATED_PATTERNS = ((128, 1), (512, 4), (2048, 16))
MAX_WINDOW = max(w for w, _ in DILATED_PATTERNS)
ATTN_BLOCK = 128
SSM_GROUP_CH = 16
SSM_GROUPS = D_SSM // SSM_GROUP_CH
SSM_STATE = 64
DT_MIN = 1e-3
DT_MAX = 1e-1
N_EXPERT_GROUPS = 4
EXPERTS_PER_GROUP = 8
N_EXPERTS = N_EXPERT_GROUPS * EXPERTS_PER_GROUP
TOP_K_FINE = 2
D_EXPERT = 512
MOE_BLOCK = 128
D_IN_PROJ = 3 * D_ATTN + D_SSM
EPS = 1e-6

kernel_name = 'hymba_dilated_s5_hmoe_step'


def rms_norm(x, g):
    xf = x.astype(jnp.float32)
    y = xf * lax.rsqrt(jnp.mean(xf * xf, axis=-1, keepdims=True) + EPS)
    return y * g.astype(jnp.float32)


def adaln(c, w, b):
    m = jnp.einsum('bd,de->be', jax.nn.silu(c.astype(jnp.float32)), w.astype(jnp.float32)) + b.astype(jnp.float32)
    shift, scale, gate = jnp.split(m[:, None, :], 3, axis=-1)
    return shift, scale, gate


def _attend(s, v, spec):
    m = jnp.max(s, axis=-1, keepdims=True)
    p = jnp.exp(s - m)
    l = jnp.sum(p, axis=-1)
    o = jnp.einsum(spec, p, v.astype(jnp.float32)) / l[..., None]
    return o, m[..., 0] + jnp.log(l)


def _combine_branches(outs, lses):
    w = jax.nn.softmax(jnp.stack(lses, axis=0), axis=0)
    return jnp.sum(w[..., None] * jnp.stack(outs, axis=0), axis=0)


def _to_subsequences(t, dil):
    b, s, h, dh = t.shape
    return t.reshape(b, s // dil, dil, h, dh).transpose(0, 2, 1, 3, 4).reshape(b * dil, s // dil, h, dh)


def _from_subsequences(t, b, dil):
    n, ls = t.shape[:2]
    rest = t.shape[2:]
    t = t.reshape((b, dil, ls) + rest)
    t = jnp.moveaxis(t, 1, 2)
    return t.reshape((b, ls * dil) + rest)


def _banded_subsequence_attention(q, k, v, n_keys):
    n, L, h, dh = q.shape
    blk = ATTN_BLOCK
    nblk = -(-L // blk)
    lp = nblk * blk
    qb = jnp.pad(q, ((0, 0), (0, lp - L), (0, 0), (0, 0))).reshape(n, nblk, blk, h, dh)

    def band(t):
        tp = jnp.pad(t, ((0, 0), (blk, lp - L), (0, 0), (0, 0)))
        prev = tp[:, :lp].reshape(n, nblk, blk, h, dh)
        cur = tp[:, blk:].reshape(n, nblk, blk, h, dh)
        return jnp.concatenate([prev, cur], axis=2)

    kb = band(k)
    vb = band(v)
    qq = np.arange(blk)[:, None]
    kk = np.arange(2 * blk)[None, :]
    dist = qq + blk - kk
    key_pos = np.arange(nblk)[:, None, None] * blk + kk[None] - blk
    mask = (dist >= 0) & (dist < n_keys) & (key_pos >= 0)
    s = jnp.einsum('nbqhd,nbkhd->nbqhk', qb, kb, preferred_element_type=jnp.float32) * (HEAD_DIM ** -0.5)
    s = jnp.where(jnp.asarray(mask)[None, :, :, None, :], s, -jnp.inf)
    o, lse = _attend(s, vb, 'nbqhk,nbkhd->nbqhd')
    return o.reshape(n, lp, h, dh)[:, :L], lse.reshape(n, lp, h)[:, :L]


def dilated_attention_prompt(q, k, v):
    b = q.shape[0]
    outs, lses = [], []
    for window, dil in DILATED_PATTERNS:
        o, lse = _banded_subsequence_attention(_to_subsequences(q, dil), _to_subsequences(k, dil),
                                               _to_subsequences(v, dil), window // dil + 1)
        outs.append(_from_subsequences(o, b, dil))
        lses.append(_from_subsequences(lse, b, dil))
    return _combine_branches(outs, lses)


def dilated_attention_sample(q, k, v, k_buf, v_buf):
    b, t, h, dh = q.shape
    w = k_buf.shape[1]
    k_ext = jnp.concatenate([k_buf.astype(k.dtype), k], axis=1)
    v_ext = jnp.concatenate([v_buf.astype(v.dtype), v], axis=1)
    outs, lses = [], []
    for window, dil in DILATED_PATTERNS:
        nk = window // dil + 1
        idx = w + np.arange(t)[:, None] - dil * np.arange(nk)[None, :]
        valid = jnp.asarray(idx >= 0)
        flat = jnp.asarray(np.clip(idx, 0, None).reshape(-1))
        kg = jnp.take(k_ext, flat, axis=1).reshape(b, t, nk, h, dh)
        vg = jnp.take(v_ext, flat, axis=1).reshape(b, t, nk, h, dh)
        s = jnp.einsum('bthd,btkhd->bthk', q, kg, preferred_element_type=jnp.float32) * (HEAD_DIM ** -0.5)
        s = jnp.where(valid[None, :, None, :], s, -jnp.inf)
        o, lse = _attend(s, vg, 'bthk,btkhd->bthd')
        outs.append(o)
        lses.append(lse)
    return _combine_branches(outs, lses)


def _scan_combine(e1, e2):
    a1, b1 = e1
    a2, b2 = e2
    return a1 * a2, a2 * b1 + b2


def s5_mixer(u, h0_re, h0_im, lam_re, lam_im, log_dt, b_re, b_im, c_re, c_im, d, w_glu, b_glu):
    f32 = jnp.float32
    bn, sn = u.shape[:2]
    lam = lax.complex(lam_re.astype(f32), lam_im.astype(f32))
    dt = jnp.exp(log_dt.astype(f32))[:, None]
    a_bar = jnp.exp(lam * dt)
    b_bar = ((a_bar - 1.0) / lam)[..., None] * lax.complex(b_re.astype(f32), b_im.astype(f32))
    ug = u.astype(f32).reshape(bn, sn, SSM_GROUPS, SSM_GROUP_CH)
    bu = jnp.einsum('gpc,bsgc->bsgp', b_bar, ug.astype(jnp.complex64))
    a_seq = jnp.broadcast_to(a_bar, (1, sn) + a_bar.shape)
    a_cum, hs = lax.associative_scan(_scan_combine, (a_seq, bu), axis=1)
    if h0_re is not None:
        hs = hs + a_cum * lax.complex(h0_re.astype(f32), h0_im.astype(f32))[:, None]
    c_mat = lax.complex(c_re.astype(f32), c_im.astype(f32))
    y = jnp.einsum('gcp,bsgp->bsgc', c_mat, hs).real + d.astype(f32) * ug
    z = jax.nn.gelu(y.reshape(bn, sn, D_SSM))
    out = z * jax.nn.sigmoid(jnp.einsum('bsc,ce->bse', z, w_glu.astype(f32)) + b_glu.astype(f32))
    h_last = hs[:, -1]
    return out, h_last.real, h_last.imag


def grouped_swiglu(xf, eid, gates, w_gate, w_up, w_down):
    t, dm = xf.shape
    n = t * TOP_K_FINE
    flat_e = eid.reshape(-1).astype(jnp.int32)
    flat_tok = jnp.repeat(jnp.arange(t, dtype=jnp.int32), TOP_K_FINE)
    flat_g = gates.reshape(-1)
    order = jnp.argsort(flat_e)
    se = flat_e[order]
    stok = flat_tok[order]
    sg = flat_g[order]
    counts = jnp.zeros((N_EXPERTS,), jnp.int32).at[flat_e].add(1)
    start = jnp.cumsum(counts) - counts
    padded = (counts + MOE_BLOCK - 1) // MOE_BLOCK * MOE_BLOCK
    pend = jnp.cumsum(padded)
    pstart = pend - padded
    dest = pstart[se] + (jnp.arange(n, dtype=jnp.int32) - start[se])
    nblk = -(-n // MOE_BLOCK) + N_EXPERTS
    xs = jnp.zeros((nblk * MOE_BLOCK, dm), xf.dtype).at[dest].set(xf[stok])
    blk_e = jnp.minimum(jnp.searchsorted(pend, jnp.arange(nblk, dtype=jnp.int32) * MOE_BLOCK, side='right'),
                        N_EXPERTS - 1)

    def expert_block(args):
        xb, e = args
        hid = jax.nn.silu(xb @ w_gate[e]) * (xb @ w_up[e])
        return hid @ w_down[e]

    ys = lax.map(expert_block, (xs.reshape(nblk, MOE_BLOCK, dm), blk_e)).reshape(nblk * MOE_BLOCK, dm)
    contrib = ys[dest].astype(jnp.float32) * sg[:, None]
    return jax.ops.segment_sum(contrib, stok, num_segments=t)


def hier_moe(h, w_rc, b_rc, w_rf, b_rf, w_gate, w_up, w_down):
    f32 = jnp.float32
    t = h.shape[0]
    hf = h.astype(f32)
    p_coarse = jax.nn.softmax(hf @ w_rc.astype(f32) + b_rc.astype(f32), axis=-1)
    grp = jnp.argmax(p_coarse, axis=-1)
    p_grp = jnp.max(p_coarse, axis=-1)
    logits_f = (hf @ w_rf.astype(f32) + b_rf.astype(f32)).reshape(t, N_EXPERT_GROUPS, EXPERTS_PER_GROUP)
    lf = logits_f[jnp.arange(t), grp]
    top_v, top_i = lax.top_k(lf, TOP_K_FINE)
    gates = p_grp[:, None] * jax.nn.softmax(top_v, axis=-1)
    eid = grp[:, None] * EXPERTS_PER_GROUP + top_i
    return grouped_swiglu(h, eid, gates, w_gate, w_up, w_down)


def layer_forward(x, c, attn_fn, h0_re, h0_im, lw):
    dt = x.dtype
    bn, sn, _ = x.shape
    shift, scale, gate = adaln(c, lw['w_ada_mix'], lw['b_ada_mix'])
    h = (rms_norm(x, lw['g_mix']) * (1.0 + scale) + shift).astype(dt)
    proj = jnp.einsum('bsd,de->bse', h, lw['w_in'])
    q, k, v, u = jnp.split(proj, [D_ATTN, 2 * D_ATTN, 3 * D_ATTN], axis=-1)
    q = q.reshape(bn, sn, N_HEADS, HEAD_DIM)
    k = k.reshape(bn, sn, N_HEADS, HEAD_DIM)
    v = v.reshape(bn, sn, N_HEADS, HEAD_DIM)
    o_attn = attn_fn(q, k, v).reshape(bn, sn, D_ATTN)
    o_ssm, hl_re, hl_im = s5_mixer(u, h0_re, h0_im, lw['lam_re'], lw['lam_im'], lw['log_dt'], lw['b_re'],
                                   lw['b_im'], lw['c_re'], lw['c_im'], lw['d'], lw['w_glu'], lw['b_glu'])
    merged = jnp.concatenate([rms_norm(o_attn, lw['g_attn_out']), rms_norm(o_ssm, lw['g_ssm_out'])],
                             axis=-1).astype(dt)
    x = (x + gate * jnp.einsum('bse,ed->bsd', merged, lw['w_out'])).astype(dt)
    shift, scale, gate = adaln(c, lw['w_ada_ffn'], lw['b_ada_ffn'])
    h = (rms_norm(x, lw['g_ffn']) * (1.0 + scale) + shift).astype(dt)
    y = hier_moe(h.reshape(bn * sn, D_MODEL), lw['w_rc'], lw['b_rc'], lw['w_rf'], lw['b_rf'],
                 lw['w_gate'], lw['w_up'], lw['w_down']).reshape(bn, sn, D_MODEL)
    x = (x + gate * y).astype(dt)
    return x, k, v, hl_re, hl_im


def setup_inputs(seed: int = 0) -> dict:
    key = jax.random.key(seed)
    ks = iter(jax.random.split(key, 48))
    f32 = jnp.float32

    def nrm(shape, s):
        return jax.random.normal(next(ks), shape, f32) * s

    L = DEPTH
    D = D_MODEL
    wbuf = min(MAX_WINDOW, PAST_LEN)
    n_idx = jnp.arange(SSM_STATE, dtype=f32)
    return {
        'x_prompt': nrm((BATCH, SEQ, D), 1.0),
        'x_sample': nrm((DEC_BATCH, DEC_SEQ, D), 1.0),
        'cache_k_win': nrm((L, DEC_BATCH, wbuf, N_HEADS, HEAD_DIM), 1.0),
        'cache_v_win': nrm((L, DEC_BATCH, wbuf, N_HEADS, HEAD_DIM), 1.0),
        'state_ssm_re': nrm((L, DEC_BATCH, SSM_GROUPS, SSM_STATE), 0.5),
        'state_ssm_im': nrm((L, DEC_BATCH, SSM_GROUPS, SSM_STATE), 0.5),
        'c_prompt': nrm((BATCH, D), 1.0),
        'c_sample': nrm((DEC_BATCH, D), 1.0),
        'g_mix': 1.0 + nrm((L, D), 0.02),
        'w_ada_mix': nrm((L, D, 3 * D), 0.5 * D ** -0.5),
        'b_ada_mix': nrm((L, 3 * D), 0.02),
        'w_in': nrm((L, D, D_IN_PROJ), D ** -0.5),
        'w_out': nrm((L, D_ATTN + D_SSM, D), (D_ATTN + D_SSM) ** -0.5),
        'g_attn_out': 1.0 + nrm((L, D_ATTN), 0.02),
        'g_ssm_out': 1.0 + nrm((L, D_SSM), 0.02),
        'ssm_lambda_re': -0.5 + nrm((L, SSM_GROUPS, SSM_STATE), 0.01),
        'ssm_lambda_im': math.pi * n_idx + nrm((L, SSM_GROUPS, SSM_STATE), 0.01),
        'ssm_log_dt': jax.random.uniform(next(ks), (L, SSM_GROUPS), f32, math.log(DT_MIN), math.log(DT_MAX)),
        'ssm_b_re': nrm((L, SSM_GROUPS, SSM_STATE, SSM_GROUP_CH), (2 * SSM_GROUP_CH) ** -0.5),
        'ssm_b_im': nrm((L, SSM_GROUPS, SSM_STATE, SSM_GROUP_CH), (2 * SSM_GROUP_CH) ** -0.5),
        'ssm_c_re': nrm((L, SSM_GROUPS, SSM_GROUP_CH, SSM_STATE), (2 * SSM_STATE) ** -0.5),
        'ssm_c_im': nrm((L, SSM_GROUPS, SSM_GROUP_CH, SSM_STATE), (2 * SSM_STATE) ** -0.5),
        'ssm_d': nrm((L, SSM_GROUPS, SSM_GROUP_CH), 0.5),
        'w_glu': nrm((L, D_SSM, D_SSM), D_SSM ** -0.5),
        'b_glu': nrm((L, D_SSM), 0.02),
        'g_ffn': 1.0 + nrm((L, D), 0.02),
        'w_ada_ffn': nrm((L, D, 3 * D), 0.5 * D ** -0.5),
        'b_ada_ffn': nrm((L, 3 * D), 0.02),
        'w_router_coarse': nrm((L, D, N_EXPERT_GROUPS), D ** -0.5),
        'b_router_coarse': nrm((L, N_EXPERT_GROUPS), 0.01),
        'w_router_fine': nrm((L, D, N_EXPERTS), D ** -0.5),
        'b_router_fine': nrm((L, N_EXPERTS), 0.01),
        'w_expert_gate': nrm((L, N_EXPERTS, D, D_EXPERT), D ** -0.5),
        'w_expert_up': nrm((L, N_EXPERTS, D, D_EXPERT), D ** -0.5),
        'w_expert_down': nrm((L, N_EXPERTS, D_EXPERT, D), D_EXPERT ** -0.5),
        'g_final': 1.0 + nrm((D,), 0.02),
    }


def reference(x_prompt, x_sample, cache_k_win, cache_v_win, state_ssm_re, state_ssm_im, c_prompt, c_sample,
              g_mix, w_ada_mix, b_ada_mix, w_in, w_out, g_attn_out, g_ssm_out,
              ssm_lambda_re, ssm_lambda_im, ssm_log_dt, ssm_b_re, ssm_b_im, ssm_c_re, ssm_c_im, ssm_d,
              w_glu, b_glu, g_ffn, w_ada_ffn, b_ada_ffn,
              w_router_coarse, b_router_coarse, w_router_fine, b_router_fine,
              w_expert_gate, w_expert_up, w_expert_down, g_final):
    keep = min(MAX_WINDOW, x_prompt.shape[1])
    xp = x_prompt
    xs = x_sample
    kp_rows, vp_rows, srp, sip = [], [], [], []
    ks_rows, vs_rows, srs, sis = [], [], [], []
    for l in range(DEPTH):
        lw = {
            'g_mix': g_mix[l], 'w_ada_mix': w_ada_mix[l], 'b_ada_mix': b_ada_mix[l],
            'w_in': w_in[l], 'w_out': w_out[l], 'g_attn_out': g_attn_out[l], 'g_ssm_out': g_ssm_out[l],
            'lam_re': ssm_lambda_re[l], 'lam_im': ssm_lambda_im[l], 'log_dt': ssm_log_dt[l],
            'b_re': ssm_b_re[l], 'b_im': ssm_b_im[l], 'c_re': ssm_c_re[l], 'c_im': ssm_c_im[l], 'd': ssm_d[l],
            'w_glu': w_glu[l], 'b_glu': b_glu[l],
            'g_ffn': g_ffn[l], 'w_ada_ffn': w_ada_ffn[l], 'b_ada_ffn': b_ada_ffn[l],
            'w_rc': w_router_coarse[l], 'b_rc': b_router_coarse[l],
            'w_rf': w_router_fine[l], 'b_rf': b_router_fine[l],
            'w_gate': w_expert_gate[l], 'w_up': w_expert_up[l], 'w_down': w_expert_down[l],
        }
        xp, kp, vp, hr, hi = layer_forward(xp, c_prompt, dilated_attention_prompt, None, None, lw)
        kp_rows.append(kp[:, kp.shape[1] - keep:])
        vp_rows.append(vp[:, vp.shape[1] - keep:])
        srp.append(hr)
        sip.append(hi)
        attn_s = functools.partial(dilated_attention_sample, k_buf=cache_k_win[l], v_buf=cache_v_win[l])
        xs, ksn, vsn, hr, hi = layer_forward(xs, c_sample, attn_s, state_ssm_re[l], state_ssm_im[l], lw)
        ks_rows.append(ksn)
        vs_rows.append(vsn)
        srs.append(hr)
        sis.append(hi)
    y_prompt = rms_norm(xp, g_final).astype(x_prompt.dtype)
    y_sample = rms_norm(xs, g_final).astype(x_sample.dtype)
    return (y_prompt, y_sample, jnp.stack(kp_rows), jnp.stack(vp_rows), jnp.stack(srp), jnp.stack(sip),
            jnp.stack(ks_rows), jnp.stack(vs_rows), jnp.stack(srs), jnp.stack(sis))
```

```python
import math
import numpy as np
from contextlib import ExitStack
import concourse.bass as bass
import concourse.mybir as mybir
from concourse.bass_utils import run_bass_kernel_spmd

F32 = mybir.dt.float32
BF16 = mybir.dt.bfloat16
I32 = mybir.dt.int32
AF = mybir.ActivationFunctionType
ALU = mybir.AluOpType
AX = mybir.AxisListType

NCORES = 8
D = 1024
S = 2048
NSEQ = 4
NSB = 16
TS = 4
NST = NSB * TS
NB = NSEQ + NSB
NH = 8
DH = 64
WBUF = 2048
EPS = 1e-6
NE = 32
DE = 512
TC = 128
DILS = (1, 4, 16)


class Res:
    __slots__ = ("name", "w", "r", "dsem", "dcount")

    def __init__(self, name):
        self.name = name
        self.w = []
        self.r = []
        self.dsem = None
        self.dcount = 0


def _merge(toks):
    best = {}
    for sem, val in toks:
        k = id(sem)
        if k not in best or best[k][1] < val:
            best[k] = (sem, val)
    return list(best.values())


class Prog:
    def __init__(self, nc, es):
        self.nc = nc
        self.es = es
        self.engs = {"pe": nc.tensor, "dve": nc.vector, "act": nc.scalar, "pool": nc.gpsimd, "sp": nc.sync}
        self.sem = {e: es.enter_context(nc.semaphore("s_" + e)) for e in ("pe", "dve", "act", "pool")}
        self.cnt = {e: 0 for e in self.sem}
        self.known = {e: {} for e in self.engs}
        self.all_dma = []
        self.dma_sems = []
        self.nres = 0

    def res(self, name=None):
        self.nres += 1
        return Res(name or f"r{self.nres}")

    def _wait(self, e, toks, skip_sem=None):
        kn = self.known[e]
        for sem, val in _merge(toks):
            if skip_sem is not None and sem is skip_sem:
                continue
            k = id(sem)
            if kn.get(k, 0) < val:
                self.engs[e].wait_ge(sem, val)
                kn[k] = val

    def _deps(self, reads, writes):
        toks = []
        for r in reads:
            toks += r.w
        for w in writes:
            toks += w.w
            toks += w.r
        return toks

    def _commit(self, tok, reads, writes):
        for r in reads:
            r.r = _merge(r.r + [tok])
        for w in writes:
            w.w = [tok]
            w.r = []

    def op(self, e, fn, reads=(), writes=()):
        reads = [getattr(x, "r", x) for x in reads]
        writes = [getattr(x, "r", x) for x in writes]
        self._wait(e, self._deps(reads, writes), skip_sem=self.sem["pe"] if e == "pe" else None)
        ins = fn(self.engs[e])
        self.cnt[e] += 1
        ins.then_inc(self.sem[e], 1)
        self._commit((self.sem[e], self.cnt[e]), reads, writes)

    def dma(self, q, out, in_, dres, reads=(), writes=(), par=False, **kw):
        dres = getattr(dres, "r", dres)
        reads = [getattr(x, "r", x) for x in reads]
        writes = [getattr(x, "r", x) for x in writes]
        if dres.dsem is None:
            if self.dma_sems:
                dres.dsem, dres.dcount = self.dma_sems.pop()
            else:
                dres.dsem = self.es.enter_context(self.nc.semaphore("d%d" % len(self.all_dma)))
            self.all_dma.append(dres)
        toks = self._deps(reads, writes)
        if par:
            toks = [t for t in toks if t[0] is not dres.dsem]
        self._wait(q, toks)
        ins = self.engs[q].dma_start(out=out, in_=in_, **kw)
        dres.dcount += 16
        ins.then_inc(dres.dsem, 16)
        tok = (dres.dsem, dres.dcount)
        for r in reads:
            r.r = _merge(r.r + [tok])
        for w in writes:
            w.w = _merge(w.w + [tok]) if par else [tok]
            if not par:
                w.r = []

    def _all_toks(self):
        toks = [(self.sem[e], self.cnt[e]) for e in self.sem if self.cnt[e] > 0]
        toks += [(r.dsem, r.dcount) for r in self.all_dma if r.dsem is not None and r.dcount > 0]
        toks += [(s, c) for s, c in self.dma_sems if c > 0]
        return toks

    def barrier(self, recycle=()):
        toks = self._all_toks()
        for e in self.engs:
            self._wait(e, toks)
        for r in recycle:
            r = getattr(r, "r", r)
            if r.dsem is not None:
                self.dma_sems.append((r.dsem, r.dcount))
                r.dsem = None
                if r in self.all_dma:
                    self.all_dma.remove(r)

    def finish(self):
        self._wait("sp", self._all_toks())


class TT:
    def __init__(self, ap, res):
        self.ap = ap
        self.r = res

    def __getitem__(self, k):
        return self.ap[k]


class Arena:
    def __init__(self, P, es, nbytes):
        self.P = P
        self.n = nbytes // 2
        self.t = es.enter_context(P.nc.sbuf_tensor("arena", [128, self.n], BF16))
        self.top = 0
        self.live = []

    def alloc(self, name, shape, dtype):
        nel = 1
        for s in shape[1:]:
            nel *= s
        units = nel * (2 if dtype in (F32, I32) else 1)
        units = (units + 31) // 32 * 32
        off = self.top
        assert off + units <= self.n, f"arena overflow allocating {name}: {off}+{units} > {self.n}"
        self.top += units
        ap = self.t[0:shape[0], off:off + units]
        if dtype in (F32, I32):
            ap = ap.bitcast(dtype)
        ap = ap[:, 0:nel]
        if len(shape) == 3:
            ap = ap.rearrange("p (a b) -> p a b", a=shape[1])
        elif len(shape) == 4:
            ap = ap.rearrange("p (a b c) -> p a b c", a=shape[1], b=shape[2])
        t = TT(ap, self.P.res(name))
        self.live.append(t)
        return t

    def mark(self):
        return (self.top, len(self.live))

    def release(self, m):
        dead = self.live[m[1]:]
        self.P.barrier(recycle=dead)
        self.live = self.live[:m[1]]
        self.top = m[0]


def build(nseq=NSEQ, stage=4, stop=None, lim=99):
    nc = bass.Bass("TRN2", target_bir_lowering=False)

    declared = []

    def din(name, shape, dt=F32):
        declared.append(name)
        return nc.dram_tensor(name, list(shape), dt, kind="ExternalInput").ap()

    def dout(name, shape, dt=F32):
        return nc.dram_tensor(name, list(shape), dt, kind="ExternalOutput").ap()

    def dscr(name, shape, dt=F32):
        return nc.dram_tensor(name, list(shape), dt, kind="Internal").ap()

    xp = din("xp", [NSEQ, S, D])
    xs = din("xs", [NST, D])
    cvec = din("cvec", [NB, D])
    w_ada_mix = din("w_ada_mix", [D, 3 * D]); b_ada_mix = din("b_ada_mix", [1, 3 * D]); g_mix = din("g_mix", [1, D])
    w_ada_ffn = din("w_ada_ffn", [D, 3 * D]); b_ada_ffn = din("b_ada_ffn", [1, 3 * D]); g_ffn = din("g_ffn", [1, D])
    g_final = din("g_final", [1, D])
    w_in = din("w_in", [D, 2048])
    w_out = din("w_out", [D, D])
    gA = din("gA", [DH, NH])
    gA2 = din("gA2", [128, 4])
    gS = din("gS", [128, 4])
    w_glu = din("w_glu", [512, 512]); bglu = din("bglu", [128, 4])
    lamre = din("lamre", [128, 16]); lamim = din("lamim", [128, 16]); logdt = din("logdt", [128, 16])
    Bre = din("Bre", [128, 16, 128]); Bim = din("Bim", [128, 16, 128])
    Cre = din("Cre", [128, 16, 128]); Cim = din("Cim", [128, 16, 128])
    Dcol = din("Dcol", [128, 4])
    h0re = din("h0re", [128, 16, NSB]); h0im = din("h0im", [128, 16, NSB])
    if stage >= 3:
        ck = din("ck", [NSB, WBUF, 512]); cv = din("cv", [NSB, WBUF, 512])
        w_eg = din("w_eg", [NE, D, DE]); w_eu = din("w_eu", [NE, D, DE]); w_ed = din("w_ed", [NE, DE, D])
    w_rt = din("w_rt", [D, 36]); b_rt = din("b_rt", [1, 36])
    ident = din("ident", [128, 128])
    selp = din("selp", [NB, NSEQ, 128]); sels = din("sels", [NB, NST])
    mask2 = din("mask2", [128, 256])
    mnew = din("mnew", [NST, NST])
    mask1s = din("mask1s", [128, TS])

    y_p = dout("y_p", [NSEQ, S, D]); y_s = dout("y_s", [NST, D])
    k_out_p = dout("k_out_p", [NSEQ, S, 512]); v_out_p = dout("v_out_p", [NSEQ, S, 512])
    k_out_s = dout("k_out_s", [NST, 512]); v_out_s = dout("v_out_s", [NST, 512])
    ssm_p_re = dout("ssm_p_re", [NSEQ, 128, 16]); ssm_p_im = dout("ssm_p_im", [NSEQ, 128, 16])
    ssm_s_re = dout("ssm_s_re", [128, 16, NSB]); ssm_s_im = dout("ssm_s_im", [128, 16, NSB])

    m_scr = dscr("m_scr", [2, NB, 3 * D])
    x1_scr = dscr("x1_scr", [S, D])
    tab_scr = dscr("tab_scr", [2, 128, 16 * TC])
    cp_scr = dscr("cp_scr", [2, 128, 16 * 128], BF16)

    with ExitStack() as es:
        P = Prog(nc, es)
        A = Arena(P, es, 194 * 1024)
        small = ExitStack(); es.enter_context(small)

        def sb(name, shape, dt):
            return TT(small.enter_context(nc.sbuf_tensor(name, shape, dt))[:], P.res(name))

        def ps(name, shape, dt):
            return TT(small.enter_context(nc.psum_tensor(name, shape, dt))[:], P.res(name))

        ident_f = sb("ident_f", [128, 128], F32)
        ident_b = sb("ident_b", [128, 128], BF16)
        ones_f = sb("ones_f", [128, 128], F32)
        ones_b = sb("ones_b", [128, 128], BF16)
        selp_t = sb("selp_t", [NB, NSEQ, 128], F32)
        sels_t = sb("sels_t", [NB, NST], F32)
        mask_b = sb("mask_b", [128, 256], BF16)
        mnew_t = sb("mnew_t", [NST, NST], F32)
        mask1s_t = sb("mask1s_t", [128, TS], F32)
        gA_t = sb("gA_t", [DH, NH], F32); gS_t = sb("gS_t", [128, 4], F32); gA2_t = sb("gA2_t", [128, 4], F32)
        bglu_t = sb("bglu_t", [128, 4], F32); Dcol_t = sb("Dcol_t", [128, 4], F32)
        brt_t = sb("brt_t", [1, 36], F32)
        cst = {k: sb("c_" + k, [128, 16], F32) for k in
               ("r", "cE", "sE", "nsE", "cr", "ci", "nci", "ncr", "ar", "ai", "nai", "c127", "s127", "icr", "ici", "nici")}
        psum = [ps(f"ps{i}", [128, 512], F32) for i in range(5)]
        psT = ps("psT", [128, 8, 128], BF16)
        psT32 = [ps("psT32a", [128, 4, 128], F32), ps("psT32b", [128, 4, 128], F32)]

        def load(t, src, q="sp"):
            P.dma(q, t.ap, src, t, writes=[t])

        for t, src in ((ident_f, ident), (selp_t, selp), (sels_t, sels), (mnew_t, mnew), (mask1s_t, mask1s), (gA_t, gA), (gA2_t, gA2),
                       (gS_t, gS), (bglu_t, bglu), (Dcol_t, Dcol), (brt_t, b_rt)):
            load(t, src)
        P.dma("pool", mask_b.ap, mask2, mask_b, writes=[mask_b])
        P.op("dve", lambda e: e.tensor_copy(out=ident_b.ap, in_=ident_f.ap), reads=[ident_f], writes=[ident_b])
        P.op("pool", lambda e: e.memset(ones_f.ap, 1.0), writes=[ones_f])
        P.op("pool", lambda e: e.memset(ones_b.ap, 1.0), writes=[ones_b])

        cnt_alt = [0]

        def evac(out_ap, in_ap, reads, writes, eng=None):
            if eng is None:
                eng = "act" if cnt_alt[0] % 2 == 0 else "dve"
                cnt_alt[0] += 1
            if eng == "act":
                P.op("act", lambda e: e.copy(out=out_ap, in_=in_ap), reads=reads, writes=writes)
            else:
                P.op(eng, lambda e: e.tensor_copy(out=out_ap, in_=in_ap), reads=reads, writes=writes)

        def adaln(w_ap, b_ap, g_ap, slot):
            m = A.mark()
            c_t = A.alloc("c_t", [NB, D], F32); sc_t = A.alloc("sc_t", [NB, D], F32)
            scT = A.alloc("scT", [128, 8, NB], F32)
            brow = A.alloc("brow", [1, 3 * D], F32); grow = A.alloc("grow", [1, D], F32)
            Mt = A.alloc("Mt", [NB, 3 * D], F32)
            wbuf = [A.alloc(f"wada{i}", [128, 8, 512], F32) for i in range(2)]
            load(c_t, cvec); load(brow, b_ap); load(grow, g_ap)
            P.op("act", lambda e: e.activation(out=sc_t.ap, in_=c_t.ap, func=AF.Silu), reads=[c_t], writes=[sc_t])
            pt = psum[0]
            for dc in range(8):
                P.op("pe", lambda e, dc=dc: e.transpose(out=pt[:, dc * NB:(dc + 1) * NB], in_=sc_t[:, dc * 128:(dc + 1) * 128],
                                                       identity=ident_f[0:NB, 0:NB]), reads=[sc_t, ident_f], writes=[pt])
            P.op("dve", lambda e: e.tensor_copy(out=scT.ap.rearrange("p a b -> p (a b)"), in_=pt[:, 0:8 * NB]), reads=[pt], writes=[scT])
            wv = w_ap.rearrange("(dc p) e -> p dc e", p=128)
            for cb in range(6):
                wb = wbuf[cb % 2]
                P.dma("sp", wb.ap, wv[:, :, cb * 512:(cb + 1) * 512], wb, writes=[wb])
                po = psum[1 + cb % 2]
                P.op("pe", lambda e, po=po, cb=cb: e.matmul(po[0:NB, :], lhsT=ones_f[0:1, 0:NB], rhs=brow[0:1, cb * 512:(cb + 1) * 512],
                                                         start=True, stop=False), reads=[ones_f, brow], writes=[po])
                for dc in range(8):
                    P.op("pe", lambda e, po=po, wb=wb, dc=dc: e.matmul(po[0:NB, :], lhsT=scT[:, dc, :], rhs=wb[:, dc, :],
                                                                   start=False, stop=(dc == 7)), reads=[scT, wb], writes=[po])
                P.op("dve", lambda e, po=po, cb=cb: e.tensor_copy(out=Mt[:, cb * 512:(cb + 1) * 512], in_=po[0:NB, :]), reads=[po], writes=[Mt])
            for hh in range(2):
                po = psum[3 + hh]
                P.op("pe", lambda e, po=po, hh=hh: e.matmul(po[0:NB, :], lhsT=ones_f[0:1, 0:NB], rhs=grow[0:1, hh * 512:(hh + 1) * 512],
                                                         start=True, stop=True), reads=[ones_f, grow], writes=[po])
                P.op("dve", lambda e, po=po, hh=hh: e.scalar_tensor_tensor(
                    out=Mt[:, D + hh * 512:D + (hh + 1) * 512], in0=Mt[:, D + hh * 512:D + (hh + 1) * 512], scalar=1.0,
                    in1=po[0:NB, :], op0=ALU.add, op1=ALU.mult), reads=[po, Mt], writes=[Mt])
            P.dma("sp", m_scr[slot], Mt.ap, Mt, reads=[Mt])
            A.release(m)

        adaln(w_ada_mix, b_ada_mix, g_mix, 0)
        adaln(w_ada_ffn, b_ada_ffn, g_ffn, 1)

        def ssm_setup():
            m = A.mark()
            f = lambda n: A.alloc(n, [128, 16], F32)
            lre, lim, ldt = f("lre"), f("lim"), f("ldt")
            load(lre, lamre); load(lim, lamim); load(ldt, logdt)
            dt, th, t0, t1, c, s, c2, s2, den, am1 = (f(n) for n in ("dt", "th", "t0", "t1", "c", "s", "c2", "s2", "den", "am1"))

            def V(fn, reads, writes):
                P.op("dve", fn, reads=reads, writes=writes)

            def TTop(out, a, b, op):
                V(lambda e: e.tensor_tensor(out=out.ap, in0=a.ap, in1=b.ap, op=op), [a, b], [out])

            def TS_(out, a, s1, op0, s2_=None, op1=None):
                if op1 is None:
                    V(lambda e: e.tensor_scalar(out=out.ap, in0=a.ap, scalar1=s1, scalar2=None, op0=op0), [a], [out])
                else:
                    V(lambda e: e.tensor_scalar(out=out.ap, in0=a.ap, scalar1=s1, scalar2=s2_, op0=op0, op1=op1), [a], [out])

            P.op("act", lambda e: e.activation(out=dt.ap, in_=ldt.ap, func=AF.Exp), reads=[ldt], writes=[dt])
            TTop(th, lim, dt, ALU.mult)
            TTop(t0, lre, dt, ALU.mult)
            P.op("act", lambda e: e.activation(out=cst["r"].ap, in_=t0.ap, func=AF.Exp), reads=[t0], writes=[cst["r"]])
            P.op("act", lambda e: e.activation(out=t1.ap, in_=th.ap, func=AF.Sin, scale=1.0 / 32), reads=[th], writes=[t1])
            TTop(c, t1, t1, ALU.mult)
            TS_(c, c, -2.0, ALU.mult, 1.0, ALU.add)
            P.op("act", lambda e: e.activation(out=s.ap, in_=th.ap, func=AF.Sin, scale=1.0 / 16), reads=[th], writes=[s])

            def square():
                TTop(c2, c, c, ALU.mult); TTop(s2, s, s, ALU.mult)
                TTop(t0, c, s, ALU.mult)
                TTop(c, c2, s2, ALU.subtract)
                TS_(s, t0, 2.0, ALU.mult)

            for _ in range(4):
                square()
            TTop(cst["ar"], cst["r"], c, ALU.mult); TTop(cst["ai"], cst["r"], s, ALU.mult)
            TS_(cst["nai"], cst["ai"], -1.0, ALU.mult)
            TS_(am1, cst["ar"], -1.0, ALU.add)
            TTop(den, lre, lre, ALU.mult); TTop(t0, lim, lim, ALU.mult); TTop(den, den, t0, ALU.add)
            V(lambda e: e.reciprocal(out=den.ap, in_=den.ap), [den], [den])
            TTop(t0, am1, lre, ALU.mult); TTop(t1, cst["ai"], lim, ALU.mult); TTop(t0, t0, t1, ALU.add)
            TTop(cst["cr"], t0, den, ALU.mult)
            TTop(t0, cst["ai"], lre, ALU.mult); TTop(t1, am1, lim, ALU.mult); TTop(t0, t0, t1, ALU.subtract)
            TTop(cst["ci"], t0, den, ALU.mult)
            TS_(cst["nci"], cst["ci"], -1.0, ALU.mult); TS_(cst["ncr"], cst["cr"], -1.0, ALU.mult)
            TTop(t0, cst["cr"], cst["cr"], ALU.mult); TTop(t1, cst["ci"], cst["ci"], ALU.mult); TTop(t0, t0, t1, ALU.add)
            V(lambda e: e.reciprocal(out=t0.ap, in_=t0.ap), [t0], [t0])
            TTop(cst["icr"], cst["cr"], t0, ALU.mult); TTop(cst["nici"], cst["ci"], t0, ALU.mult)
            TS_(cst["ici"], cst["nici"], -1.0, ALU.mult)
            cosT = A.alloc("cosT", [128, 16, TC], F32); sinT = A.alloc("sinT", [128, 16, TC], F32)
            tA = A.alloc("tA", [128, 16, TC // 2], F32); tB = A.alloc("tB", [128, 16, TC // 2], F32)
            P.op("pool", lambda e: e.memset(cosT[:, :, 0:1], 1.0), writes=[cosT])
            P.op("pool", lambda e: e.memset(sinT[:, :, 0:1], 0.0), writes=[sinT])
            n = 1
            while n < TC:
                cb_ = c.ap.unsqueeze(2).to_broadcast([128, 16, n]); sb_ = s.ap.unsqueeze(2).to_broadcast([128, 16, n])
                V(lambda e, n=n, cb_=cb_: e.tensor_tensor(out=tA[:, :, 0:n], in0=cosT[:, :, 0:n], in1=cb_, op=ALU.mult), [cosT, c], [tA])
                V(lambda e, n=n, sb_=sb_: e.tensor_tensor(out=tB[:, :, 0:n], in0=sinT[:, :, 0:n], in1=sb_, op=ALU.mult), [sinT, s], [tB])
                V(lambda e, n=n: e.tensor_tensor(out=cosT[:, :, n:2 * n], in0=tA[:, :, 0:n], in1=tB[:, :, 0:n], op=ALU.subtract), [tA, tB, cosT], [cosT])
                V(lambda e, n=n, sb_=sb_: e.tensor_tensor(out=tA[:, :, 0:n], in0=cosT[:, :, 0:n], in1=sb_, op=ALU.mult), [cosT, s], [tA])
                V(lambda e, n=n, cb_=cb_: e.tensor_tensor(out=tB[:, :, 0:n], in0=sinT[:, :, 0:n], in1=cb_, op=ALU.mult), [sinT, c], [tB])
                V(lambda e, n=n: e.tensor_tensor(out=sinT[:, :, n:2 * n], in0=tA[:, :, 0:n], in1=tB[:, :, 0:n], op=ALU.add), [tA, tB, sinT], [sinT])
                square()
                n *= 2
            V(lambda e: e.tensor_copy(out=cst["cE"].ap, in_=c.ap), [c], [cst["cE"]])
            V(lambda e: e.tensor_copy(out=cst["sE"].ap, in_=s.ap), [s], [cst["sE"]])
            TS_(cst["nsE"], s, -1.0, ALU.mult)
            V(lambda e: e.tensor_copy(out=cst["c127"].ap, in_=cosT[:, :, TC - 1]), [cosT], [cst["c127"]])
            V(lambda e: e.tensor_copy(out=cst["s127"].ap, in_=sinT[:, :, TC - 1]), [sinT], [cst["s127"]])
            P.dma("sp", tab_scr[0], cosT.ap.rearrange("p a b -> p (a b)"), cosT, reads=[cosT])
            P.dma("sp", tab_scr[1], sinT.ap.rearrange("p a b -> p (a b)"), sinT, reads=[sinT])
            Cr = A.alloc("Cr", [128, 16, 128], F32); Ci = A.alloc("Ci", [128, 16, 128], F32)
            tmp = A.alloc("tmpC", [128, 128], F32)
            Cpr = A.alloc("Cpr", [128, 16, 128], BF16); Cpi = A.alloc("Cpi", [128, 16, 128], BF16)
            load(Cr, Cre); load(Ci, Cim)
            for j in range(16):
                V(lambda e, j=j: e.tensor_scalar(out=tmp.ap, in0=Cr[:, j, :], scalar1=cst["cr"][:, j:j + 1], scalar2=None, op0=ALU.mult),
                  [Cr, cst["cr"]], [tmp])
                V(lambda e, j=j: e.scalar_tensor_tensor(out=Cpr[:, j, :], in0=Ci[:, j, :], scalar=cst["nci"][:, j:j + 1], in1=tmp.ap,
                                                        op0=ALU.mult, op1=ALU.add), [Ci, cst["nci"], tmp], [Cpr])
                V(lambda e, j=j: e.tensor_scalar(out=tmp.ap, in0=Cr[:, j, :], scalar1=cst["nci"][:, j:j + 1], scalar2=None, op0=ALU.mult),
                  [Cr, cst["nci"]], [tmp])
                V(lambda e, j=j: e.scalar_tensor_tensor(out=Cpi[:, j, :], in0=Ci[:, j, :], scalar=cst["ncr"][:, j:j + 1], in1=tmp.ap,
                                                        op0=ALU.mult, op1=ALU.add), [Ci, cst["ncr"], tmp], [Cpi])
            P.dma("sp", cp_scr[0], Cpr.ap.rearrange("p a b -> p (a b)"), Cpr, reads=[Cpr])
            P.dma("sp", cp_scr[1], Cpi.ap.rearrange("p a b -> p (a b)"), Cpi, reads=[Cpi])
            A.release(m)

        ssm_setup()

        def make_bc(bc, slot, sel_fn, npart, cbs):
            m = A.mark()
            Mt = A.alloc("Mt", [NB, 3 * D], F32)
            load(Mt, m_scr[slot])
            for i, cb in enumerate(cbs):
                po = psum[i % 2]
                P.op("pe", lambda e, po=po, cb=cb: e.matmul(po[0:npart, :], lhsT=sel_fn(), rhs=Mt[:, cb * 512:(cb + 1) * 512],
                                                         start=True, stop=True), reads=[Mt, selp_t, sels_t], writes=[po])
                evac(bc[0:npart, i * 512:(i + 1) * 512], po[0:npart, :], [po], [bc])
            A.release(m)

        def rstd_from_ss(rstd, ss, npart, n):
            P.op("act", lambda e: e.activation(out=rstd[0:npart, :], in_=ss[0:npart, :], func=AF.Sqrt, scale=1.0 / n, bias=EPS),
                 reads=[ss], writes=[rstd])
            P.op("dve", lambda e: e.reciprocal(out=rstd[0:npart, :], in_=rstd[0:npart, :]), reads=[rstd], writes=[rstd])

        def norm_to_hT(hT, x_src, sel_fn, npart, ntiles):
            m = A.mark()
            bc = A.alloc("bc", [128, 2 * D], F32)
            make_bc(bc, 0, sel_fn, npart, [0, 1, 2, 3])
            xbuf = [A.alloc(f"xb{i}", [128, D], F32) for i in range(2)]
            junk = A.alloc("junk", [128, D], BF16)
            ss = A.alloc("ss", [128, 1], F32); rstd = A.alloc("rstd", [128, 1], F32)
            t1 = A.alloc("t1", [128, D], F32); hb = A.alloc("hb", [128, D], BF16)
            for ti in range(ntiles):
                xt = xbuf[ti % 2]
                P.dma("sp", xt[0:npart, :], x_src(ti), xt, writes=[xt])
                P.op("act", lambda e, xt=xt: e.activation(out=junk[0:npart, :], in_=xt[0:npart, :], func=AF.Square, accum_out=ss[0:npart, :]),
                     reads=[xt], writes=[junk, ss])
                rstd_from_ss(rstd, ss, npart, D)
                P.op("dve", lambda e, xt=xt: e.scalar_tensor_tensor(out=t1[0:npart, :], in0=xt[0:npart, :], scalar=rstd[0:npart, 0:1],
                                                                   in1=bc[0:npart, D:2 * D], op0=ALU.mult, op1=ALU.mult),
                     reads=[xt, rstd, bc], writes=[t1])
                P.op("dve", lambda e: e.tensor_tensor(out=hb[0:npart, :], in0=t1[0:npart, :], in1=bc[0:npart, 0:D], op=ALU.add),
                     reads=[t1, bc], writes=[hb])
                for dc in range(8):
                    P.op("pe", lambda e, dc=dc: e.transpose(out=psT[:, dc, 0:npart], in_=hb[0:npart, dc * 128:(dc + 1) * 128],
                                                           identity=ident_b[0:npart, 0:npart]), reads=[hb, ident_b], writes=[psT])
                P.op("act", lambda e, ti=ti: e.copy(out=hT[:, :, ti * npart:(ti + 1) * npart], in_=psT[:, :, 0:npart]), reads=[psT], writes=[hT])
            A.release(m)

        w_in_v = w_in.rearrange("(dc p) e -> p dc e", p=128)

        def load_wblk(wb, cb):
            for dc in range(8):
                P.dma("pool", wb[:, dc, :], w_in_v[:, dc, cb * 512:(cb + 1) * 512], wb, writes=[wb], par=(dc > 0))

        def proj_fm(dst, wb, hT, ntb, tbw):
            for tb in range(ntb):
                for ec in range(4):
                    po = psum[(tb * 4 + ec) % 4]
                    for dc in range(8):
                        P.op("pe", lambda e, po=po, dc=dc, ec=ec, tb=tb: e.matmul(
                            po[:, 0:tbw], lhsT=wb[:, dc, ec * 128:(ec + 1) * 128], rhs=hT[:, dc, tb * tbw:(tb + 1) * tbw],
                            start=(dc == 0), stop=(dc == 7)), reads=[wb, hT], writes=[po])
                    evac(dst[:, ec, tb * tbw:(tb + 1) * tbw], po[:, 0:tbw], [po], [dst])

        def ssm_prompt(gi, uT):
            m = A.mark()
            cosT = A.alloc("cosT", [128, 16, TC], F32); sinT = A.alloc("sinT", [128, 16, TC], F32)
            Bpr = A.alloc("Bpr", [128, 16, 128], BF16); Bpi = A.alloc("Bpi", [128, 16, 128], BF16)
            Cpr = A.alloc("Cpr", [128, 16, 128], BF16); Cpi = A.alloc("Cpi", [128, 16, 128], BF16)
            wg = A.alloc("wglu", [128, 4, 512], BF16)
            load(cosT, tab_scr[0].rearrange("p (a b) -> p a b", a=16)); load(sinT, tab_scr[1].rearrange("p (a b) -> p a b", a=16))
            load(Cpr, cp_scr[0].rearrange("p (a b) -> p a b", a=16)); load(Cpi, cp_scr[1].rearrange("p (a b) -> p a b", a=16))
            P.dma("pool", Bpr.ap, Bre, Bpr, writes=[Bpr]); P.dma("pool", Bpi.ap, Bim, Bpi, writes=[Bpi])
            P.dma("pool", wg.ap, w_glu.rearrange("(cc p) e -> p cc e", p=128), wg, writes=[wg])
            W = 512
            NCH = W // TC
            f = lambda n: A.alloc(n, [128, W], F32)
            m1, m2, m3, m4, br, bi, gr, gi_, ysb = (f(n) for n in ("m1", "m2", "m3", "m4", "br", "bi", "gr", "gi", "ysb"))
            hr = A.alloc("hr", [128, W], BF16); hi = A.alloc("hi", [128, W], BF16)
            zT = A.alloc("zT", [128, 4, W], BF16); sig = A.alloc("sig", [128, W], BF16)
            glast = A.alloc("glast", [128, 16, 2], F32)
            g0 = A.alloc("g0", [128, 4], F32)
            fin = A.alloc("fin", [128, 4, 16], F32)
            pX = [psum[0], psum[1]]; pY = [psum[2], psum[3]]; pG = psum[4]
            v3 = lambda t: t.ap.rearrange("p (a b) -> p a b", a=NCH)
            for tb in range(S // W):
                tsl = slice(tb * W, (tb + 1) * W)
                for j in range(16):
                    cc = j // 4
                    P.op("pe", lambda e, j=j, cc=cc, tsl=tsl: e.matmul(pX[0][:, :], lhsT=Bpr[:, j, :], rhs=uT[:, cc, tsl], start=True, stop=True),
                         reads=[Bpr, uT], writes=[pX[0]])
                    P.op("pe", lambda e, j=j, cc=cc, tsl=tsl: e.matmul(pX[1][:, :], lhsT=Bpi[:, j, :], rhs=uT[:, cc, tsl], start=True, stop=True),
                         reads=[Bpi, uT], writes=[pX[1]])
                    cosb = cosT[:, j, :].unsqueeze(1).to_broadcast([128, NCH, TC]); sinb = sinT[:, j, :].unsqueeze(1).to_broadcast([128, NCH, TC])
                    P.op("dve", lambda e, cosb=cosb: e.tensor_tensor(out=v3(m1), in0=v3(pX[0]), in1=cosb, op=ALU.mult), reads=[pX[0], cosT], writes=[m1])
                    P.op("dve", lambda e, sinb=sinb: e.tensor_tensor(out=v3(m2), in0=v3(pX[1]), in1=sinb, op=ALU.mult), reads=[pX[1], sinT], writes=[m2])
                    P.op("dve", lambda e, cosb=cosb: e.tensor_tensor(out=v3(m3), in0=v3(pX[1]), in1=cosb, op=ALU.mult), reads=[pX[1], cosT], writes=[m3])
                    P.op("dve", lambda e, sinb=sinb: e.tensor_tensor(out=v3(m4), in0=v3(pX[0]), in1=sinb, op=ALU.mult), reads=[pX[0], sinT], writes=[m4])
                    P.op("pool", lambda e: e.tensor_tensor(out=br.ap, in0=m1.ap, in1=m2.ap, op=ALU.add), reads=[m1, m2], writes=[br])
                    P.op("pool", lambda e: e.tensor_tensor(out=bi.ap, in0=m3.ap, in1=m4.ap, op=ALU.subtract), reads=[m3, m4], writes=[bi])
                    for ch in range(NCH):
                        csl = slice(ch * TC, (ch + 1) * TC)
                        if tb == 0 and ch == 0:
                            ir, ii = 0.0, 0.0
                        else:
                            if ch == 0:
                                lr, li = glast[:, j, 0:1], glast[:, j, 1:2]
                            else:
                                lr, li = gr[:, ch * TC - 1:ch * TC], gi_[:, ch * TC - 1:ch * TC]
                            P.op("dve", lambda e, lr=lr, j=j: e.tensor_scalar(out=g0[:, 0:1], in0=lr, scalar1=cst["cE"][:, j:j + 1], scalar2=None, op0=ALU.mult),
                                 reads=[gr, glast, cst["cE"]], writes=[g0])
                            P.op("dve", lambda e, li=li, j=j: e.scalar_tensor_tensor(out=g0[:, 0:1], in0=li, scalar=cst["nsE"][:, j:j + 1], in1=g0[:, 0:1], op0=ALU.mult, op1=ALU.add),
                                 reads=[gi_, glast, cst["nsE"], g0], writes=[g0])
                            P.op("dve", lambda e, li=li, j=j: e.tensor_scalar(out=g0[:, 1:2], in0=li, scalar1=cst["cE"][:, j:j + 1], scalar2=None, op0=ALU.mult),
                                 reads=[gi_, glast, cst["cE"]], writes=[g0])
                            P.op("dve", lambda e, lr=lr, j=j: e.scalar_tensor_tensor(out=g0[:, 1:2], in0=lr, scalar=cst["sE"][:, j:j + 1], in1=g0[:, 1:2], op0=ALU.mult, op1=ALU.add),
                                 reads=[gr, glast, cst["sE"], g0], writes=[g0])
                            ir, ii = g0[:, 0:1], g0[:, 1:2]
                        rb = cst["r"][:, j:j + 1].to_broadcast([128, TC])
                        P.op("dve", lambda e, csl=csl, rb=rb, ir=ir: e.tensor_tensor_scan(out=gr[:, csl], data0=rb, data1=br[:, csl], initial=ir, op0=ALU.mult, op1=ALU.add),
                             reads=[br, cst["r"], g0], writes=[gr])
                        P.op("dve", lambda e, csl=csl, rb=rb, ii=ii: e.tensor_tensor_scan(out=gi_[:, csl], data0=rb, data1=bi[:, csl], initial=ii, op0=ALU.mult, op1=ALU.add),
                             reads=[bi, cst["r"], g0], writes=[gi_])
                    P.op("dve", lambda e, j=j: e.tensor_copy(out=glast[:, j, 0:1], in_=gr[:, W - 1:W]), reads=[gr], writes=[glast])
                    P.op("dve", lambda e, j=j: e.tensor_copy(out=glast[:, j, 1:2], in_=gi_[:, W - 1:W]), reads=[gi_], writes=[glast])
                    P.op("dve", lambda e, cosb=cosb: e.tensor_tensor(out=v3(m1), in0=v3(gr), in1=cosb, op=ALU.mult), reads=[gr, cosT], writes=[m1])
                    P.op("pool", lambda e, sinb=sinb: e.tensor_tensor(out=v3(m2), in0=v3(gi_), in1=sinb, op=ALU.mult), reads=[gi_, sinT], writes=[m2])
                    P.op("dve", lambda e, sinb=sinb: e.tensor_tensor(out=v3(m3), in0=v3(gr), in1=sinb, op=ALU.mult), reads=[gr, sinT], writes=[m3])
                    P.op("pool", lambda e, cosb=cosb: e.tensor_tensor(out=v3(m4), in0=v3(gi_), in1=cosb, op=ALU.mult), reads=[gi_, cosT], writes=[m4])
                    P.op("pool", lambda e: e.tensor_tensor(out=hr.ap, in0=m1.ap, in1=m2.ap, op=ALU.subtract), reads=[m1, m2], writes=[hr])
                    P.op("pool", lambda e: e.tensor_tensor(out=hi.ap, in0=m3.ap, in1=m4.ap, op=ALU.add), reads=[m3, m4], writes=[hi])
                    py = pY[cc % 2]
                    P.op("pe", lambda e, py=py, j=j: e.matmul(py[:, :], lhsT=Cpr[:, j, :], rhs=hr.ap, start=(j % 4 == 0), stop=False), reads=[Cpr, hr], writes=[py])
                    P.op("pe", lambda e, py=py, j=j: e.matmul(py[:, :], lhsT=Cpi[:, j, :], rhs=hi.ap, start=False, stop=(j % 4 == 3)), reads=[Cpi, hi], writes=[py])
                    if j % 4 == 3:
                        P.op("dve", lambda e, py=py, cc=cc, tsl=tsl: e.scalar_tensor_tensor(out=ysb.ap, in0=uT[:, cc, tsl], scalar=Dcol_t[:, cc:cc + 1], in1=py[:, :],
                                                                                         op0=ALU.mult, op1=ALU.add), reads=[uT, Dcol_t, py], writes=[ysb])
                        P.op("act", lambda e, cc=cc: e.activation(out=zT[:, cc, :], in_=ysb.ap, func=AF.Gelu), reads=[ysb], writes=[zT])
                for ec in range(4):
                    for cc in range(4):
                        P.op("pe", lambda e, ec=ec, cc=cc: e.matmul(pG[:, :], lhsT=wg[:, cc, ec * 128:(ec + 1) * 128], rhs=zT[:, cc, :], start=(cc == 0), stop=(cc == 3)),
                             reads=[wg, zT], writes=[pG])
                    P.op("act", lambda e, ec=ec: e.activation(out=sig.ap, in_=pG[:, :], func=AF.Sigmoid, bias=bglu_t[:, ec:ec + 1]), reads=[pG, bglu_t], writes=[sig])
                    P.op("pool", lambda e, ec=ec, tsl=tsl: e.tensor_tensor(out=uT[:, ec, tsl], in0=zT[:, ec, :], in1=sig.ap, op=ALU.mult), reads=[zT, sig], writes=[uT])
            glr = glast[:, :, 0]; gli = glast[:, :, 1]
            F = lambda k: fin[:, k, :]

            def V(fn, reads, writes):
                P.op("dve", fn, reads=reads, writes=writes)
            V(lambda e: e.tensor_tensor(out=F(0), in0=glr, in1=cst["c127"].ap, op=ALU.mult), [glast, cst["c127"]], [fin])
            V(lambda e: e.tensor_tensor(out=F(1), in0=gli, in1=cst["s127"].ap, op=ALU.mult), [glast, cst["s127"]], [fin])
            V(lambda e: e.tensor_tensor(out=F(0), in0=F(0), in1=F(1), op=ALU.subtract), [fin], [fin])
            V(lambda e: e.tensor_tensor(out=F(1), in0=glr, in1=cst["s127"].ap, op=ALU.mult), [glast, cst["s127"]], [fin])
            V(lambda e: e.tensor_tensor(out=F(2), in0=gli, in1=cst["c127"].ap, op=ALU.mult), [glast, cst["c127"]], [fin])
            V(lambda e: e.tensor_tensor(out=F(1), in0=F(1), in1=F(2), op=ALU.add), [fin], [fin])
            V(lambda e: e.tensor_tensor(out=F(2), in0=F(0), in1=cst["cr"].ap, op=ALU.mult), [fin, cst["cr"]], [fin])
            V(lambda e: e.tensor_tensor(out=F(3), in0=F(1), in1=cst["ci"].ap, op=ALU.mult), [fin, cst["ci"]], [fin])
            V(lambda e: e.tensor_tensor(out=F(2), in0=F(2), in1=F(3), op=ALU.subtract), [fin], [fin])
            V(lambda e: e.tensor_tensor(out=F(3), in0=F(0), in1=cst["ci"].ap, op=ALU.mult), [fin, cst["ci"]], [fin])
            V(lambda e: e.tensor_tensor(out=F(0), in0=F(1), in1=cst["cr"].ap, op=ALU.mult), [fin, cst["cr"]], [fin])
            V(lambda e: e.tensor_tensor(out=F(3), in0=F(3), in1=F(0), op=ALU.add), [fin], [fin])
            P.dma("sp", ssm_p_re[gi], F(2), fin, reads=[fin])
            P.dma("sp", ssm_p_im[gi], F(3), fin, reads=[fin])
            A.release(m)

        def attn_prompt(hT, qT, kT, wv_b, oA):
            m = A.mark()
            Vp = [A.alloc(f"Vp{d}", [128, 16, NH, DH + 1], BF16) for d in DILS]
            AccN = A.alloc("AccN", [DH + 1, S], F32)
            PT = [A.alloc(f"PT{i}", [128, 256], BF16) for i in range(2)]
            for di, d in enumerate(DILS):
                nblk = 16 // d
                P.op("pool", lambda e, di=di: e.memset(Vp[di][:, :, :, DH:DH + 1], 1.0), writes=[Vp[di]])
                for r in range(d):
                    for jb in range(nblk):
                        blk = r * nblk + jb
                        k0 = r + d * 128 * jb
                        sl = slice(k0, k0 + 127 * d + 1, d)
                        po = psum[blk % 4]
                        for dc in range(8):
                            P.op("pe", lambda e, po=po, dc=dc, sl=sl: e.matmul(po[:, :], lhsT=hT[:, dc, sl], rhs=wv_b[:, dc, :], start=(dc == 0), stop=(dc == 7)),
                                 reads=[hT, wv_b], writes=[po])
                        evac(Vp[di][:, blk, :, 0:DH], po[:, :].rearrange("p (h d) -> p h d", h=NH), [po], [Vp[di]])
            it = 0
            for h in range(NH):
                hp, p0 = h // 2, (h % 2) * DH
                P.op("pool", lambda e: e.memset(AccN.ap, 0.0), writes=[AccN])
                for di, d in enumerate(DILS):
                    nblk = 16 // d
                    for r in range(d):
                        for kb in range(nblk):
                            blk = r * nblk + kb
                            k0 = r + d * 128 * kb
                            nq = 256 if kb + 1 < nblk else 128
                            ksl = slice(k0, k0 + 127 * d + 1, d); qsl = slice(k0, k0 + (nq - 1) * d + 1, d)
                            pS = psum[it % 2]; pO = psum[2 + it % 2]; pt = PT[it % 2]
                            it += 1
                            P.op("pe", lambda e, pS=pS, ksl=ksl, qsl=qsl, nq=nq, hp=hp, p0=p0: e.matmul(
                                pS[:, 0:nq], lhsT=kT[p0:p0 + DH, hp, ksl], rhs=qT[p0:p0 + DH, hp, qsl], start=True, stop=True),
                                reads=[kT, qT], writes=[pS])
                            P.op("act", lambda e, pS=pS, pt=pt, nq=nq: e.activation(out=pt[:, 0:nq], in_=pS[:, 0:nq], func=AF.Exp, scale=DH ** -0.5),
                                 reads=[pS], writes=[pt])
                            P.op("pool", lambda e, pt=pt, nq=nq: e.tensor_tensor(out=pt[:, 0:nq], in0=pt[:, 0:nq], in1=mask_b[:, 0:nq], op=ALU.mult),
                                 reads=[pt, mask_b], writes=[pt])
                            P.op("pe", lambda e, pO=pO, pt=pt, nq=nq, di=di, blk=blk, h=h: e.matmul(
                                pO[0:DH + 1, 0:nq], lhsT=Vp[di][:, blk, h, :], rhs=pt[:, 0:nq], start=True, stop=True),
                                reads=[Vp[di], pt], writes=[pO])
                            P.op("dve", lambda e, pO=pO, qsl=qsl, nq=nq: e.tensor_tensor(out=AccN[:, qsl], in0=AccN[:, qsl], in1=pO[0:DH + 1, 0:nq], op=ALU.add),
                                 reads=[AccN, pO], writes=[AccN])
                P.op("dve", lambda e: e.reciprocal(out=AccN[DH:DH + 1, :], in_=AccN[DH:DH + 1, :]), reads=[AccN], writes=[AccN])
                for tb in range(4):
                    pB = psum[4]
                    P.op("pe", lambda e, tb=tb: e.matmul(pB[0:DH, :], lhsT=ones_f[DH:DH + 1, 0:DH], rhs=AccN[DH:DH + 1, tb * 512:(tb + 1) * 512], start=True, stop=True),
                         reads=[ones_f, AccN], writes=[pB])
                    P.op("dve", lambda e, tb=tb, h=h: e.tensor_tensor(out=oA[:, h, tb * 512:(tb + 1) * 512], in0=AccN[0:DH, tb * 512:(tb + 1) * 512], in1=pB[0:DH, :], op=ALU.mult),
                         reads=[AccN, pB], writes=[oA])
            A.release(m)

        def ssm_sample(uT):
            m = A.mark()
            Bpr = A.alloc("Bpr", [128, 16, 128], BF16); Bpi = A.alloc("Bpi", [128, 16, 128], BF16)
            Cpr = A.alloc("Cpr", [128, 16, 128], BF16); Cpi = A.alloc("Cpi", [128, 16, 128], BF16)
            wg = A.alloc("wglu", [128, 4, 512], BF16)
            load(Cpr, cp_scr[0].rearrange("p (a b) -> p a b", a=16)); load(Cpi, cp_scr[1].rearrange("p (a b) -> p a b", a=16))
            P.dma("pool", Bpr.ap, Bre, Bpr, writes=[Bpr]); P.dma("pool", Bpi.ap, Bim, Bpi, writes=[Bpi])
            P.dma("pool", wg.ap, w_glu.rearrange("(cc p) e -> p cc e", p=128), wg, writes=[wg])
            f3 = lambda n: A.alloc(n, [128, 16, NSB], F32)
            H0r, H0i, hpr, hpi, t1, t2, t3, t4 = (f3(n) for n in ("H0r", "H0i", "hpr", "hpi", "t1", "t2", "t3", "t4"))
            Xr = A.alloc("Xr", [128, 16, NST], F32); Xi = A.alloc("Xi", [128, 16, NST], F32)
            Hr = A.alloc("Hr", [128, 16, NST], BF16); Hi = A.alloc("Hi", [128, 16, NST], BF16)
            ysb = A.alloc("ysb", [128, NST], F32); zT = A.alloc("zT", [128, 4, NST], BF16); sig = A.alloc("sig", [128, NST], BF16)
            load(H0r, h0re); load(H0i, h0im)
            bcs = lambda k: cst[k].ap.unsqueeze(2).to_broadcast([128, 16, NSB])

            def V(fn, reads, writes):
                P.op("dve", fn, reads=reads, writes=writes)

            def mul(out, a, k):
                V(lambda e: e.tensor_tensor(out=out.ap, in0=a.ap, in1=bcs(k), op=ALU.mult), [a, cst[k]], [out])

            def comb(out, a, b, op):
                V(lambda e: e.tensor_tensor(out=out.ap, in0=a.ap, in1=b.ap, op=op), [a, b], [out])

            mul(t1, H0r, "icr"); mul(t2, H0i, "ici"); mul(t3, H0r, "ici"); mul(t4, H0i, "icr")
            comb(hpr, t1, t2, ALU.subtract); comb(hpi, t3, t4, ALU.add)
            for j in range(16):
                cc = j // 4
                pa, pb = psum[j % 2], psum[2 + j % 2]
                P.op("pe", lambda e, j=j, cc=cc, pa=pa: e.matmul(pa[:, 0:NST], lhsT=Bpr[:, j, :], rhs=uT[:, cc, :], start=True, stop=True), reads=[Bpr, uT], writes=[pa])
                P.op("pe", lambda e, j=j, cc=cc, pb=pb: e.matmul(pb[:, 0:NST], lhsT=Bpi[:, j, :], rhs=uT[:, cc, :], start=True, stop=True), reads=[Bpi, uT], writes=[pb])
                evac(Xr[:, j, :], pa[:, 0:NST], [pa], [Xr]); evac(Xi[:, j, :], pb[:, 0:NST], [pb], [Xi])
            xv = lambda X, t: X.ap.rearrange("p j (b t) -> p j b t", t=TS)[:, :, :, t]
            for t in range(TS):
                mul(t1, hpr, "ar"); mul(t2, hpi, "ai"); mul(t3, hpr, "ai"); mul(t4, hpi, "ar")
                comb(t1, t1, t2, ALU.subtract); comb(t3, t3, t4, ALU.add)
                V(lambda e, t=t: e.tensor_tensor(out=hpr.ap, in0=t1.ap, in1=xv(Xr, t), op=ALU.add), [t1, Xr], [hpr])
                V(lambda e, t=t: e.tensor_tensor(out=hpi.ap, in0=t3.ap, in1=xv(Xi, t), op=ALU.add), [t3, Xi], [hpi])
                V(lambda e, t=t: e.tensor_copy(out=xv(Hr, t), in_=hpr.ap), [hpr], [Hr])
                V(lambda e, t=t: e.tensor_copy(out=xv(Hi, t), in_=hpi.ap), [hpi], [Hi])
            mul(t1, hpr, "cr"); mul(t2, hpi, "ci"); mul(t3, hpr, "ci"); mul(t4, hpi, "cr")
            comb(t1, t1, t2, ALU.subtract); comb(t3, t3, t4, ALU.add)
            P.dma("sp", ssm_s_re, t1.ap, t1, reads=[t1]); P.dma("sp", ssm_s_im, t3.ap, t3, reads=[t3])
            for cc in range(4):
                py = psum[cc % 2]
                for jj in range(4):
                    j = cc * 4 + jj
                    P.op("pe", lambda e, py=py, j=j, jj=jj: e.matmul(py[:, 0:NST], lhsT=Cpr[:, j, :], rhs=Hr[:, j, :], start=(jj == 0), stop=False), reads=[Cpr, Hr], writes=[py])
                    P.op("pe", lambda e, py=py, j=j, jj=jj: e.matmul(py[:, 0:NST], lhsT=Cpi[:, j, :], rhs=Hi[:, j, :], start=False, stop=(jj == 3)), reads=[Cpi, Hi], writes=[py])
                P.op("dve", lambda e, py=py, cc=cc: e.scalar_tensor_tensor(out=ysb.ap, in0=uT[:, cc, :], scalar=Dcol_t[:, cc:cc + 1], in1=py[:, 0:NST], op0=ALU.mult, op1=ALU.add),
                     reads=[uT, Dcol_t, py], writes=[ysb])
                P.op("act", lambda e, cc=cc: e.activation(out=zT[:, cc, :], in_=ysb.ap, func=AF.Gelu), reads=[ysb], writes=[zT])
            pG = psum[4]
            for ec in range(4):
                for cc in range(4):
                    P.op("pe", lambda e, ec=ec, cc=cc: e.matmul(pG[:, 0:NST], lhsT=wg[:, cc, ec * 128:(ec + 1) * 128], rhs=zT[:, cc, :], start=(cc == 0), stop=(cc == 3)),
                         reads=[wg, zT], writes=[pG])
                P.op("act", lambda e, ec=ec: e.activation(out=sig.ap, in_=pG[:, 0:NST], func=AF.Sigmoid, bias=bglu_t[:, ec:ec + 1]), reads=[pG, bglu_t], writes=[sig])
                P.op("pool", lambda e, ec=ec: e.tensor_tensor(out=uT[:, ec, :], in0=zT[:, ec, :], in1=sig.ap, op=ALU.mult), reads=[zT, sig], writes=[uT])
            A.release(m)

        def attn_sample(qT, kT, vS, qtm, oAs):
            m = A.mark()
            Kt = [A.alloc(f"Kt{i}", [128, 512], F32) for i in range(2)]
            Vt = [A.alloc(f"Vt{i}", [128, 512], F32) for i in range(2)]
            Vb = [A.alloc(f"Vb{i}", [128, NH, DH + 1], BF16) for i in range(9)]
            qb = [A.alloc(f"qb{i}", [128, 512], F32) for i in range(TS)]
            tmp = A.alloc("tmpA", [128, 512], F32)
            NCOL = 12
            Sall = A.alloc("Sall", [128, NCOL, NH], F32); Eall = A.alloc("Eall", [128, NCOL, NH], BF16)
            PTp = [A.alloc(f"PTp{i}", [128, NH, NST], BF16) for i in range(2)]
            En = A.alloc("En", [NST, NST], F32); PTn = A.alloc("PTn", [NST, NST], BF16)
            rl = A.alloc("rl", [NST, NH], F32)
            pacc = [psum[2], psum[3]]
            selq = A.alloc("selq", [NST, NST, 128], F32)
            P.op("dve", lambda e: e.tensor_copy(out=selq.ap, in_=ident_f[0:NST, 0:NST].unsqueeze(2).to_broadcast([NST, NST, 128])), reads=[ident_f], writes=[selq])
            for i in range(9):
                P.op("pool", lambda e, i=i: e.memset(Vb[i].ap, 1.0), writes=[Vb[i]])
            reg = lambda h: pacc[h // 4][0:NST, (h % 4) * (DH + 1):(h % 4 + 1) * (DH + 1)]
            P.op("pool", lambda e: e.memset(PTp[0].ap, 0.0), writes=[PTp[0]])
            for bank in range(2):
                P.op("pe", lambda e, bank=bank: e.matmul(pacc[bank][0:NST, 0:4 * (DH + 1)], lhsT=PTp[0][:, 0, :],
                                                        rhs=Vb[0].ap.rearrange("p h d -> p (h d)")[:, 0:4 * (DH + 1)], start=True, stop=False),
                     reads=[PTp[0], Vb[0]], writes=[pacc[bank]])
            for h in range(NH):
                hp, p0 = h // 2, (h % 2) * DH
                pS = psum[h % 2]
                P.op("pe", lambda e, pS=pS, hp=hp, p0=p0: e.matmul(pS[0:NST, 0:NST], lhsT=kT[p0:p0 + DH, hp, :], rhs=qT[p0:p0 + DH, hp, :], start=True, stop=True),
                     reads=[kT, qT], writes=[pS])
                P.op("act", lambda e, pS=pS: e.activation(out=En.ap, in_=pS[0:NST, 0:NST], func=AF.Exp, scale=DH ** -0.5), reads=[pS], writes=[En])
                P.op("dve", lambda e: e.tensor_tensor(out=PTn.ap, in0=En.ap, in1=mnew_t.ap, op=ALU.mult), reads=[En, mnew_t], writes=[PTn])
                P.op("pe", lambda e, h=h: e.matmul(reg(h), lhsT=PTn.ap, rhs=vS[:, h, :], start=False, stop=False), reads=[PTn, vS], writes=[pacc[h // 4]])
            it = 0
            for b in range(NSB):
                for t in range(TS):
                    tok = b * TS + t
                    pq = psum[t % 2]
                    P.op("pe", lambda e, pq=pq, tok=tok: e.matmul(pq[:, :], lhsT=selq[:, tok, :], rhs=qtm.ap, start=True, stop=True),
                         reads=[selq, qtm], writes=[pq])
                    evac(qb[t].ap, pq[:, :], [pq], [qb[t]], eng="act")
                tiles = [(ck[b, WBUF - 128:WBUF, :], cv[b, WBUF - 128:WBUF, :], list(range(TS)))]
                for t in range(TS):
                    tiles.append((ck[b, WBUF - 512 + t:WBUF - 512 + t + 509:4, :], cv[b, WBUF - 512 + t:WBUF - 512 + t + 509:4, :], [t]))
                for t in range(TS):
                    tiles.append((ck[b, t:t + 2033:16, :], cv[b, t:t + 2033:16, :], [t]))
                col = 0
                pend = []
                for (ksrc, vsrc, qs) in tiles:
                    kt, vt, vb = Kt[it % 2], Vt[it % 2], Vb[len(pend)]
                    it += 1
                    P.dma("sp", kt.ap, ksrc, kt, writes=[kt])
                    P.dma("sp", vt.ap, vsrc, vt, writes=[vt])
                    P.op("pool", lambda e, vt=vt, vb=vb: e.tensor_copy(out=vb[:, :, 0:DH], in_=vt.ap.rearrange("p (h d) -> p h d", h=NH)), reads=[vt], writes=[vb])
                    cols = []
                    for t in qs:
                        P.op("dve", lambda e, kt=kt, t=t: e.tensor_tensor(out=tmp.ap, in0=kt.ap, in1=qb[t].ap, op=ALU.mult), reads=[kt, qb[t]], writes=[tmp])
                        P.op("dve", lambda e, col=col: e.tensor_reduce(out=Sall[:, col, :], in_=tmp.ap.rearrange("p (h d) -> p h d", h=NH), axis=AX.X, op=ALU.add),
                             reads=[tmp], writes=[Sall])
                        cols.append((col, t)); col += 1
                    pend.append((vb, cols, len(qs) == TS))
                P.op("act", lambda e: e.activation(out=Eall.ap, in_=Sall.ap, func=AF.Exp, scale=DH ** -0.5), reads=[Sall], writes=[Eall])
                for ti_, (vb, cols, is_d1) in enumerate(pend):
                    ptp = PTp[ti_ % 2]
                    P.op("pool", lambda e, ptp=ptp: e.memset(ptp.ap, 0.0), writes=[ptp])
                    for (c_, t) in cols:
                        tok = b * TS + t
                        if is_d1:
                            P.op("dve", lambda e, ptp=ptp, c_=c_, t=t, tok=tok: e.tensor_scalar(out=ptp[:, :, tok], in0=Eall[:, c_, :], scalar1=mask1s_t[:, t:t + 1], scalar2=None, op0=ALU.mult),
                                 reads=[Eall, mask1s_t], writes=[ptp])
                        else:
                            P.op("dve", lambda e, ptp=ptp, c_=c_, tok=tok: e.tensor_copy(out=ptp[:, :, tok], in_=Eall[:, c_, :]), reads=[Eall], writes=[ptp])
                    last = (b == NSB - 1 and ti_ == len(pend) - 1)
                    for h in range(NH):
                        P.op("pe", lambda e, h=h, ptp=ptp, vb=vb, last=last: e.matmul(reg(h), lhsT=ptp[:, h, :], rhs=vb[:, h, :], start=False, stop=last),
                             reads=[ptp, vb], writes=[pacc[h // 4]])
            for bank in range(2):
                pv = pacc[bank][0:NST, 0:4 * (DH + 1)].rearrange("p (h d) -> p h d", h=4)
                P.op("dve", lambda e, pv=pv, bank=bank: e.reciprocal(out=rl[:, bank * 4:(bank + 1) * 4], in_=pv[:, :, DH]), reads=[pacc[bank]], writes=[rl])
                P.op("dve", lambda e, pv=pv, bank=bank: e.tensor_tensor(
                    out=oAs.ap.rearrange("p (h d) -> p h d", h=NH)[:, bank * 4:(bank + 1) * 4, :], in0=pv[:, :, 0:DH],
                    in1=rl[:, bank * 4:(bank + 1) * 4].unsqueeze(2).to_broadcast([NST, 4, DH]), op=ALU.mult), reads=[pacc[bank], rl], writes=[oAs])
            A.release(m)

        def route(lg, sm, Gt, ti, n):
            def V(fn, reads, writes):
                P.op("dve", fn, reads=reads, writes=writes)
            mx = sm[0:n, 0:1]; nmx = sm[0:n, 1:2]; se = sm[0:n, 2:3]; pg = sm[0:n, 3:4]
            ec = sm[0:n, 4:8]; oh = sm[0:n, 8:12]; lf = sm[0:n, 12:20]; mk1 = sm[0:n, 20:28]; lf2 = sm[0:n, 28:36]
            mk2 = sm[0:n, 36:44]; m1 = sm[0:n, 44:45]; m2 = sm[0:n, 45:46]; dd = sm[0:n, 46:47]; ed = sm[0:n, 47:48]
            g1 = sm[0:n, 48:49]; w1 = sm[0:n, 49:50]; w2 = sm[0:n, 50:51]; wm = sm[0:n, 52:60]
            V(lambda e: e.reduce_max(out=mx, in_=lg[0:n, 0:4], axis=AX.X), [lg], [sm])
            V(lambda e: e.tensor_scalar(out=nmx, in0=mx, scalar1=-1.0, scalar2=None, op0=ALU.mult), [sm], [sm])
            P.op("act", lambda e: e.activation(out=ec, in_=lg[0:n, 0:4], func=AF.Exp, bias=nmx, accum_out=se), reads=[lg, sm], writes=[sm])
            V(lambda e: e.reciprocal(out=pg, in_=se), [sm], [sm])
            V(lambda e: e.tensor_scalar(out=oh, in0=lg[0:n, 0:4], scalar1=mx, scalar2=None, op0=ALU.is_equal), [lg, sm], [sm])
            V(lambda e: e.tensor_scalar(out=lf, in0=lg[0:n, 4:12], scalar1=oh[:, 0:1], scalar2=None, op0=ALU.mult), [lg, sm], [sm])
            for g in range(1, 4):
                V(lambda e, g=g: e.scalar_tensor_tensor(out=lf, in0=lg[0:n, 4 + 8 * g:12 + 8 * g], scalar=oh[:, g:g + 1], in1=lf, op0=ALU.mult, op1=ALU.add),
                  [lg, sm], [sm])
            V(lambda e: e.reduce_max(out=m1, in_=lf, axis=AX.X), [sm], [sm])
            V(lambda e: e.tensor_scalar(out=mk1, in0=lf, scalar1=m1, scalar2=None, op0=ALU.is_equal), [sm], [sm])
            V(lambda e: e.scalar_tensor_tensor(out=lf2, in0=mk1, scalar=-1e30, in1=lf, op0=ALU.mult, op1=ALU.add), [sm], [sm])
            V(lambda e: e.reduce_max(out=m2, in_=lf2, axis=AX.X), [sm], [sm])
            V(lambda e: e.tensor_scalar(out=mk2, in0=lf2, scalar1=m2, scalar2=None, op0=ALU.is_equal), [sm], [sm])
            V(lambda e: e.tensor_tensor(out=dd, in0=m2, in1=m1, op=ALU.subtract), [sm], [sm])
            P.op("act", lambda e: e.activation(out=ed, in_=dd, func=AF.Exp), reads=[sm], writes=[sm])
            V(lambda e: e.tensor_scalar(out=g1, in0=ed, scalar1=1.0, scalar2=None, op0=ALU.add), [sm], [sm])
            V(lambda e: e.reciprocal(out=g1, in_=g1), [sm], [sm])
            V(lambda e: e.tensor_tensor(out=w1, in0=g1, in1=pg, op=ALU.mult), [sm], [sm])
            V(lambda e: e.tensor_tensor(out=w2, in0=w1, in1=ed, op=ALU.mult), [sm], [sm])
            V(lambda e: e.tensor_scalar(out=wm, in0=mk1, scalar1=w1, scalar2=None, op0=ALU.mult), [sm], [sm])
            V(lambda e: e.scalar_tensor_tensor(out=wm, in0=mk2, scalar=w2, in1=wm, op0=ALU.mult, op1=ALU.add), [sm], [sm])
            for g in range(4):
                V(lambda e, g=g: e.tensor_scalar(out=Gt[0:n, ti, 8 * g:8 * g + 8], in0=wm, scalar1=oh[:, g:g + 1], scalar2=None, op0=ALU.mult), [sm], [Gt])

        def outproj_ffn_norm(hT, Gt, uT, oA, x_src, sel_fn, npart, ntiles, is_sample):
            m = A.mark()
            bcg = A.alloc("bcg", [128, D], F32)
            make_bc(bcg, 0, sel_fn, npart, [4, 5])
            bc2 = A.alloc("bc2", [128, 2 * D], F32)
            make_bc(bc2, 1, sel_fn, npart, [0, 1, 2, 3])
            if is_sample:
                woA = A.alloc("woAs", [128, 4, D], BF16)
            else:
                woA = A.alloc("woA", [DH, NH, D], BF16)
            woS = A.alloc("woS", [128, 4, D], BF16)
            wrt = A.alloc("wrt", [128, 8, 36], F32)
            load(wrt, w_rt.rearrange("(dc p) e -> p dc e", p=128))
            wr_hi = A.alloc("wr_hi", [128, 8, 36], BF16); wr_lo = A.alloc("wr_lo", [128, 8, 36], BF16)
            bias_bc = A.alloc("bias_bc", [128, 36], F32)
            P.op("dve", lambda e: e.tensor_copy(out=wr_hi.ap, in_=wrt.ap), reads=[wrt], writes=[wr_hi])
            P.op("dve", lambda e: e.tensor_tensor(out=wrt.ap, in0=wrt.ap, in1=wr_hi.ap, op=ALU.subtract), reads=[wrt, wr_hi], writes=[wrt])
            P.op("dve", lambda e: e.tensor_copy(out=wr_lo.ap, in_=wrt.ap), reads=[wrt], writes=[wr_lo])
            P.op("pe", lambda e: e.matmul(psum[4][:, 0:36], lhsT=ones_f[0:1, :], rhs=brt_t[0:1, :], start=True, stop=True), reads=[ones_f, brt_t], writes=[psum[4]])
            P.op("dve", lambda e: e.tensor_copy(out=bias_bc.ap, in_=psum[4][:, 0:36]), reads=[psum[4]], writes=[bias_bc])
            mw = A.mark()
            wtmp = [A.alloc(f"wtmp{i}", [128, D], F32) for i in range(2)]
            k = 0
            if is_sample:
                for cc in range(4):
                    wt = wtmp[k % 2]; k += 1
                    P.dma("sp", wt.ap, w_out[cc * 128:(cc + 1) * 128, :], wt, writes=[wt])
                    P.op("dve", lambda e, cc=cc, wt=wt: e.tensor_scalar(out=woA[:, cc, :], in0=wt.ap, scalar1=gA2_t[:, cc:cc + 1], scalar2=None, op0=ALU.mult),
                         reads=[wt, gA2_t], writes=[woA])
            else:
                for h in range(NH):
                    wt = wtmp[k % 2]; k += 1
                    P.dma("sp", wt[0:DH, :], w_out[h * DH:(h + 1) * DH, :], wt, writes=[wt])
                    P.op("dve", lambda e, h=h, wt=wt: e.tensor_scalar(out=woA[:, h, :], in0=wt[0:DH, :], scalar1=gA_t[:, h:h + 1], scalar2=None, op0=ALU.mult),
                         reads=[wt, gA_t], writes=[woA])
            for cc in range(4):
                wt = wtmp[k % 2]; k += 1
                P.dma("sp", wt.ap, w_out[512 + cc * 128:512 + (cc + 1) * 128, :], wt, writes=[wt])
                P.op("dve", lambda e, cc=cc, wt=wt: e.tensor_scalar(out=woS[:, cc, :], in0=wt.ap, scalar1=gS_t[:, cc:cc + 1], scalar2=None, op0=ALU.mult),
                     reads=[wt, gS_t], writes=[woS])
            A.release(mw)
            if lim == 0:
                A.release(m); return
            xbuf = [A.alloc(f"xb{i}", [128, D], F32) for i in range(2)]
            acc = A.alloc("acc", [128, D], F32)
            sq = A.alloc("sq", [128, 12, 128], BF16)
            ssa = A.alloc("ssa", [128, 2], F32); rs2 = A.alloc("rs2", [128, 2], F32)
            junk = A.alloc("junk", [128, D], BF16)
            ss = A.alloc("ss", [128, 1], F32); rstd = A.alloc("rstd", [128, 1], F32)
            h2lo = A.alloc("h2lo", [128, 8, 128], BF16)
            hbf = A.alloc("hbf", [128, D], BF16); lbf = A.alloc("lbf", [128, D], BF16)
            lg = A.alloc("lg", [128, 36], F32)
            sm = A.alloc("sm", [128, 64], F32)
            if is_sample:
                oT_s = A.alloc("oT_s", [128, 4, NST], BF16)
            pa = [psum[0], psum[1]]; pss = [psum[2], psum[3]]; pq = psum[4]
            for ti in range(ntiles):
                c0 = ti * npart
                xt = xbuf[ti % 2]
                P.dma("sp", xt[0:npart, :], x_src(ti), xt, writes=[xt])
                P.op("act", lambda e, c0=c0: e.activation(out=sq[:, 8:12, 0:npart], in_=uT[:, :, c0:c0 + npart], func=AF.Square), reads=[uT], writes=[sq])
                for cc in range(4):
                    P.op("pe", lambda e, cc=cc: e.matmul(pq[0:npart, 32:64], lhsT=sq[:, 8 + cc, 0:npart], rhs=ones_b[:, 0:32], start=(cc == 0), stop=(cc == 3)),
                         reads=[sq, ones_b], writes=[pq])
                if not is_sample:
                    P.op("act", lambda e, c0=c0: e.activation(out=sq[0:DH, 0:NH, :], in_=oA[:, :, c0:c0 + 128], func=AF.Square), reads=[oA], writes=[sq])
                    for h in range(NH):
                        P.op("pe", lambda e, h=h: e.matmul(pq[:, 0:32], lhsT=sq[0:DH, h, :], rhs=ones_b[0:DH, 0:32], start=(h == 0), stop=(h == NH - 1)),
                             reads=[sq, ones_b], writes=[pq])
                    P.op("dve", lambda e: e.tensor_copy(out=ssa.ap, in_=pq[:, 0:64:32]), reads=[pq], writes=[ssa])
                else:
                    P.op("dve", lambda e: e.tensor_copy(out=ssa[0:NST, 1:2], in_=pq[0:NST, 32:33]), reads=[pq], writes=[ssa])
                    P.op("act", lambda e: e.activation(out=junk[0:NST, 0:512], in_=oA.ap, func=AF.Square, accum_out=ssa[0:NST, 0:1]), reads=[oA], writes=[junk, ssa])
                    P.op("dve", lambda e: e.tensor_copy(out=junk[0:NST, 512:1024], in_=oA.ap), reads=[oA], writes=[junk])
                    for cc in range(4):
                        P.op("pe", lambda e, cc=cc: e.transpose(out=psT[:, cc, 0:NST], in_=junk[0:NST, 512 + cc * 128:512 + (cc + 1) * 128],
                                                               identity=ident_b[0:NST, 0:NST]), reads=[junk, ident_b], writes=[psT])
                    P.op("act", lambda e: e.copy(out=oT_s.ap, in_=psT[:, 0:4, 0:NST]), reads=[psT], writes=[oT_s])
                rstd_from_ss(rs2, ssa, npart, 512)
                if lim == 1:
                    break
                for hf in range(2):
                    hsl = slice(hf * 512, (hf + 1) * 512)
                    if is_sample:
                        for cc in range(4):
                            P.op("pe", lambda e, cc=cc, hf=hf, hsl=hsl: e.matmul(pa[hf][0:NST, :], lhsT=oT_s[:, cc, :], rhs=woA[:, cc, hsl], start=(cc == 0), stop=(cc == 3)),
                                 reads=[oT_s, woA], writes=[pa[hf]])
                    else:
                        for h in range(NH):
                            P.op("pe", lambda e, h=h, hf=hf, hsl=hsl, c0=c0: e.matmul(pa[hf][:, :], lhsT=oA[:, h, c0:c0 + 128], rhs=woA[:, h, hsl], start=(h == 0), stop=(h == NH - 1)),
                                 reads=[oA, woA], writes=[pa[hf]])
                    for cc in range(4):
                        P.op("pe", lambda e, cc=cc, hf=hf, hsl=hsl, c0=c0: e.matmul(pss[hf][0:npart, :], lhsT=uT[:, cc, c0:c0 + npart], rhs=woS[:, cc, hsl], start=(cc == 0), stop=(cc == 3)),
                             reads=[uT, woS], writes=[pss[hf]])
                    P.op("dve", lambda e, hf=hf, hsl=hsl: e.tensor_scalar(out=acc[0:npart, hsl], in0=pa[hf][0:npart, :], scalar1=rs2[0:npart, 0:1], scalar2=None, op0=ALU.mult),
                         reads=[pa[hf], rs2], writes=[acc])
                    P.op("dve", lambda e, hf=hf, hsl=hsl: e.scalar_tensor_tensor(out=acc[0:npart, hsl], in0=pss[hf][0:npart, :], scalar=rs2[0:npart, 1:2], in1=acc[0:npart, hsl],
                                                                                op0=ALU.mult, op1=ALU.add), reads=[pss[hf], rs2, acc], writes=[acc])
                P.op("pool", lambda e: e.tensor_tensor(out=acc[0:npart, :], in0=acc[0:npart, :], in1=bcg[0:npart, :], op=ALU.mult), reads=[acc, bcg], writes=[acc])
                P.op("pool", lambda e, xt=xt: e.tensor_tensor(out=xt[0:npart, :], in0=acc[0:npart, :], in1=xt[0:npart, :], op=ALU.add), reads=[acc, xt], writes=[xt])
                if lim == 2:
                    break
                P.dma("sp", x1_scr[c0:c0 + npart, :], xt[0:npart, :], xt, reads=[xt])
                if lim == 3:
                    break
                P.op("act", lambda e, xt=xt: e.activation(out=junk[0:npart, :], in_=xt[0:npart, :], func=AF.Square, accum_out=ss[0:npart, :]),
                     reads=[xt], writes=[junk, ss])
                rstd_from_ss(rstd, ss, npart, D)
                P.op("dve", lambda e, xt=xt: e.scalar_tensor_tensor(out=acc[0:npart, :], in0=xt[0:npart, :], scalar=rstd[0:npart, 0:1],
                                                                   in1=bc2[0:npart, D:2 * D], op0=ALU.mult, op1=ALU.mult), reads=[xt, rstd, bc2], writes=[acc])
                P.op("dve", lambda e: e.tensor_tensor(out=acc[0:npart, :], in0=acc[0:npart, :], in1=bc2[0:npart, 0:D], op=ALU.add), reads=[acc, bc2], writes=[acc])
                P.op("act", lambda e: e.copy(out=hbf[0:npart, :], in_=acc[0:npart, :]), reads=[acc], writes=[hbf])
                P.op("dve", lambda e: e.tensor_tensor(out=acc[0:npart, :], in0=acc[0:npart, :], in1=hbf[0:npart, :], op=ALU.subtract), reads=[acc, hbf], writes=[acc])
                P.op("act", lambda e: e.copy(out=lbf[0:npart, :], in_=acc[0:npart, :]), reads=[acc], writes=[lbf])
                for dc in range(8):
                    P.op("pe", lambda e, dc=dc: e.transpose(out=psT[:, dc, 0:npart], in_=hbf[0:npart, dc * 128:(dc + 1) * 128],
                                                           identity=ident_b[0:npart, 0:npart]), reads=[hbf, ident_b], writes=[psT])
                P.op("act", lambda e, c0=c0: e.copy(out=hT[:, :, c0:c0 + npart], in_=psT[:, :, 0:npart]), reads=[psT], writes=[hT])
                for dc in range(8):
                    P.op("pe", lambda e, dc=dc: e.transpose(out=psT[:, dc, 0:npart], in_=lbf[0:npart, dc * 128:(dc + 1) * 128],
                                                           identity=ident_b[0:npart, 0:npart]), reads=[lbf, ident_b], writes=[psT])
                P.op("dve", lambda e: e.tensor_copy(out=h2lo[:, :, 0:npart], in_=psT[:, :, 0:npart]), reads=[psT], writes=[h2lo])
                if lim == 4:
                    break
                k_ = 0
                for (lt, wt_) in (("hi", wr_hi), ("hi", wr_lo), ("lo", wr_hi)):
                    for dc in range(8):
                        lhs = hT[:, dc, c0:c0 + npart] if lt == "hi" else h2lo[:, dc, 0:npart]
                        P.op("pe", lambda e, lhs=lhs, wt_=wt_, dc=dc, k_=k_: e.matmul(pq[0:npart, 0:36], lhsT=lhs, rhs=wt_[:, dc, :], start=(k_ == 0), stop=(k_ == 23)),
                             reads=[hT, h2lo, wt_], writes=[pq])
                        k_ += 1
                P.op("dve", lambda e: e.tensor_tensor(out=lg[0:npart, :], in0=pq[0:npart, 0:36], in1=bias_bc[0:npart, :], op=ALU.add), reads=[pq, bias_bc], writes=[lg])
                if lim == 5:
                    break
                route(lg, sm, Gt, ti, npart)
            A.release(m)

        def moe_final(h2T, Gt, sel_fn, npart, ntiles, ntok, y_dst):
            m = A.mark()
            tbw = min(512, ntok); ntb = ntok // tbw; tpb = tbw // npart
            yacc = A.alloc("yacc", [128, ntiles, D], F32)
            wgs = [A.alloc(f"weg{i}", [128, 8, DE], BF16) for i in range(2)]
            wus = [A.alloc(f"weu{i}", [128, 8, DE], BF16) for i in range(2)]
            wds = [A.alloc(f"wed{i}", [128, 4, D], BF16) for i in range(2)]
            hid = [A.alloc(f"hid{i}", [128, 4, 512], BF16) for i in range(2)]
            sg = [A.alloc(f"sg{i}", [128, 512], F32) for i in range(2)]
            it = 0
            for ex in range(NE):
                wg_, wu_, wd_ = wgs[ex % 2], wus[ex % 2], wds[ex % 2]
                gv = w_eg[ex].rearrange("(dc p) f -> p dc f", p=128); uv = w_eu[ex].rearrange("(dc p) f -> p dc f", p=128)
                dv = w_ed[ex].rearrange("(fc p) d -> p fc d", p=128)
                for dc in range(8):
                    P.dma("pool", wg_[:, dc, :], gv[:, dc, :], wg_, writes=[wg_], par=(dc > 0))
                for dc in range(8):
                    P.dma("pool", wu_[:, dc, :], uv[:, dc, :], wu_, writes=[wu_], par=(dc > 0))
                for fc in range(4):
                    P.dma("pool", wd_[:, fc, :], dv[:, fc, :], wd_, writes=[wd_], par=(fc > 0))
                for tb in range(ntb):
                    hd = hid[it % 2]; it += 1
                    tsl = slice(tb * tbw, (tb + 1) * tbw)
                    for fc in range(4):
                        pG, pU = psum[fc % 2], psum[2 + fc % 2]
                        fsl = slice(fc * 128, (fc + 1) * 128)
                        for dc in range(8):
                            P.op("pe", lambda e, pG=pG, dc=dc, fsl=fsl, tsl=tsl: e.matmul(pG[:, 0:tbw], lhsT=wg_[:, dc, fsl], rhs=h2T[:, dc, tsl], start=(dc == 0), stop=(dc == 7)),
                                 reads=[wg_, h2T], writes=[pG])
                        for dc in range(8):
                            P.op("pe", lambda e, pU=pU, dc=dc, fsl=fsl, tsl=tsl: e.matmul(pU[:, 0:tbw], lhsT=wu_[:, dc, fsl], rhs=h2T[:, dc, tsl], start=(dc == 0), stop=(dc == 7)),
                                 reads=[wu_, h2T], writes=[pU])
                        s_ = sg[fc % 2]
                        P.op("act", lambda e, pG=pG, s_=s_: e.activation(out=s_[:, 0:tbw], in_=pG[:, 0:tbw], func=AF.Silu), reads=[pG], writes=[s_])
                        P.op("dve", lambda e, pU=pU, s_=s_, hd=hd, fc=fc: e.tensor_tensor(out=hd[:, fc, 0:tbw], in0=s_[:, 0:tbw], in1=pU[:, 0:tbw], op=ALU.mult),
                             reads=[s_, pU], writes=[hd])
                    for tl in range(tpb):
                        T_ = tb * tpb + tl
                        for hf in range(2):
                            pD = psum[4]
                            hsl = slice(hf * 512, (hf + 1) * 512)
                            for fc in range(4):
                                P.op("pe", lambda e, fc=fc, hd=hd, tl=tl, hsl=hsl: e.matmul(pD[0:npart, :], lhsT=hd[:, fc, tl * npart:(tl + 1) * npart], rhs=wd_[:, fc, hsl],
                                                                                         start=(fc == 0), stop=(fc == 3)), reads=[hd, wd_], writes=[pD])
                            if ex == 0:
                                P.op("dve", lambda e, T_=T_, hsl=hsl, ex=ex: e.tensor_scalar(out=yacc[0:npart, T_, hsl], in0=pD[0:npart, :], scalar1=Gt[0:npart, T_, ex:ex + 1], scalar2=None, op0=ALU.mult),
                                     reads=[pD, Gt], writes=[yacc])
                            else:
                                P.op("dve", lambda e, T_=T_, hsl=hsl, ex=ex: e.scalar_tensor_tensor(out=yacc[0:npart, T_, hsl], in0=pD[0:npart, :], scalar=Gt[0:npart, T_, ex:ex + 1],
                                                                                                 in1=yacc[0:npart, T_, hsl], op0=ALU.mult, op1=ALU.add),
                                     reads=[pD, Gt, yacc], writes=[yacc])
            bcg2 = A.alloc("bcg2", [128, D], F32)
            mm = A.mark()
            wfree = None
            A.release(mm)
            make_bc(bcg2, 1, sel_fn, npart, [4, 5])
            gfb = A.alloc("gfb", [128, D], F32)
            grow = A.alloc("growf", [1, D], F32)
            load(grow, g_final)
            for hh in range(2):
                po = psum[hh]
                P.op("pe", lambda e, po=po, hh=hh: e.matmul(po[:, :], lhsT=ones_f[0:1, :], rhs=grow[0:1, hh * 512:(hh + 1) * 512], start=True, stop=True),
                     reads=[ones_f, grow], writes=[po])
                evac(gfb[:, hh * 512:(hh + 1) * 512], po[:, :], [po], [gfb])
            xbuf = [A.alloc(f"xf{i}", [128, D], F32) for i in range(2)]
            junk = A.alloc("junkf", [128, D], BF16)
            ss = A.alloc("ssf", [128, 1], F32); rstd = A.alloc("rstdf", [128, 1], F32)
            for ti in range(ntiles):
                xt = xbuf[ti % 2]
                P.dma("sp", xt[0:npart, :], x1_scr[ti * npart:(ti + 1) * npart, :], xt, writes=[xt])
                P.op("pool", lambda e, ti=ti: e.tensor_tensor(out=yacc[0:npart, ti, :], in0=yacc[0:npart, ti, :], in1=bcg2[0:npart, :], op=ALU.mult), reads=[yacc, bcg2], writes=[yacc])
                P.op("pool", lambda e, ti=ti, xt=xt: e.tensor_tensor(out=xt[0:npart, :], in0=xt[0:npart, :], in1=yacc[0:npart, ti, :], op=ALU.add), reads=[yacc, xt], writes=[xt])
                P.op("act", lambda e, xt=xt: e.activation(out=junk[0:npart, :], in_=xt[0:npart, :], func=AF.Square, accum_out=ss[0:npart, :]), reads=[xt], writes=[junk, ss])
                rstd_from_ss(rstd, ss, npart, D)
                P.op("dve", lambda e, xt=xt: e.scalar_tensor_tensor(out=xt[0:npart, :], in0=xt[0:npart, :], scalar=rstd[0:npart, 0:1], in1=gfb[0:npart, :], op0=ALU.mult, op1=ALU.mult),
                     reads=[xt, rstd, gfb], writes=[xt])
                P.dma("sp", y_dst(ti), xt[0:npart, :], xt, reads=[xt])
            A.release(m)

        def group(gi, is_sample):
            npart = NST if is_sample else 128
            ntiles = 1 if is_sample else 16
            ntok = npart * ntiles
            tbw = min(512, ntok); ntb = ntok // tbw
            sel_fn = (lambda: sels_t[:, :]) if is_sample else (lambda: selp_t[:, gi, :])
            x_src = (lambda ti: xs[:, :]) if is_sample else (lambda ti: xp[gi, ti * 128:(ti + 1) * 128, :])
            k_dst = (lambda ti: k_out_s[:, :]) if is_sample else (lambda ti: k_out_p[gi, ti * 128:(ti + 1) * 128, :])
            v_dst = (lambda ti: v_out_s[:, :]) if is_sample else (lambda ti: v_out_p[gi, ti * 128:(ti + 1) * 128, :])
            y_dst = (lambda ti: y_s[:, :]) if is_sample else (lambda ti: y_p[gi, ti * 128:(ti + 1) * 128, :])
            g0 = A.mark()
            hT = A.alloc("hT", [128, 8, ntok], BF16)
            Gt = A.alloc("Gt", [128, ntiles, NE], F32)
            g1 = A.mark()
            qT = A.alloc("qT", [128, 4, ntok], BF16)
            kT = A.alloc("kT", [128, 4, ntok], BF16)
            uT = A.alloc("uT", [128, 4, ntok], BF16)
            wv_b = A.alloc("wv_b", [128, 8, 512], BF16)
            if is_sample:
                oA = A.alloc("oAs", [NST, 512], F32)
                vS = A.alloc("vS", [NST, NH, DH + 1], BF16)
                qtm = A.alloc("qtm", [NST, 512], F32)
            else:
                oA = A.alloc("oA", [DH, NH, ntok], BF16)
            norm_to_hT(hT, x_src, sel_fn, npart, ntiles)
            m = A.mark()
            wblk = [A.alloc(f"wblk{i}", [128, 8, 512], BF16) for i in range(2)]
            kst = [A.alloc(f"kst{i}", [128, 512], F32) for i in range(2)]

            def proj_tm(wb, dst_fn, also=None, keep=None):
                for ti in range(ntiles):
                    po = psum[ti % 4]
                    for dc in range(8):
                        P.op("pe", lambda e, po=po, dc=dc, ti=ti: e.matmul(
                            po[0:npart, :], lhsT=hT[:, dc, ti * npart:(ti + 1) * npart], rhs=wb[:, dc, :],
                            start=(dc == 0), stop=(dc == 7)), reads=[hT, wb], writes=[po])
                    if keep is not None:
                        evac(keep.ap, po[0:npart, :], [po], [keep])
                        continue
                    sg_ = kst[ti % 2]
                    evac(sg_[0:npart, :], po[0:npart, :], [po], [sg_])
                    P.dma("sp", dst_fn(ti), sg_[0:npart, :], sg_, reads=[sg_])
                    if also is not None:
                        also(po)

            def _early(k):
                return is_sample and lim == k
            if _early(10):
                A.release(g0); return
            load_wblk(wblk[0], 0)
            load_wblk(wblk[1], 1)
            proj_fm(qT, wblk[0], hT, ntb, tbw)
            if _early(11):
                A.release(g0); return
            if is_sample:
                proj_tm(wblk[0], None, keep=qtm)
            if _early(12):
                A.release(g0); return
            load_wblk(wblk[0], 3)
            proj_fm(kT, wblk[1], hT, ntb, tbw)
            proj_tm(wblk[1], k_dst)
            if _early(13):
                A.release(g0); return
            load_wblk(wv_b, 2)
            proj_fm(uT, wblk[0], hT, ntb, tbw)
            if _early(14):
                A.release(g0); return
            if is_sample:
                P.op("pool", lambda e: e.memset(vS.ap, 1.0), writes=[vS])
                proj_tm(wv_b, v_dst, also=lambda po: P.op("pool", lambda e: e.tensor_copy(out=vS[:, :, 0:DH], in_=kst[0][0:NST, :].rearrange("p (h d) -> p h d", h=NH)),
                                                      reads=[kst[0]], writes=[vS]))
            else:
                proj_tm(wv_b, v_dst)
            A.release(m)
            if is_sample:
                if stop == "proj":
                    A.release(g0); return
                ssm_sample(uT)
                if stop == "ssm":
                    A.release(g0); return
                attn_sample(qT, kT, vS, qtm, oA)
            else:
                ssm_prompt(gi, uT)
                if stop == "ssm":
                    A.release(g0); return
                attn_prompt(hT, qT, kT, wv_b, oA)
            if stop == "attn":
                A.release(g0); return
            outproj_ffn_norm(hT, Gt, uT, oA, x_src, sel_fn, npart, ntiles, is_sample)
            A.release(g1)
            if stop == "outproj":
                A.release(g0); return
            moe_final(hT, Gt, sel_fn, npart, ntiles, ntok, y_dst)
            A.release(g0)

        for b in range(nseq):
            group(b, False)
        if stage >= 4:
            group(0, True)
        P.finish()
    return nc, declared


def _consts():
    ident = np.eye(128, dtype=np.float32)
    selp = np.zeros((NB, NSEQ, 128), np.float32)
    for b in range(NSEQ):
        selp[b, b, :] = 1.0
    sels = np.zeros((NB, NST), np.float32)
    for b in range(NSB):
        sels[NSEQ + b, b * TS:(b + 1) * TS] = 1.0
    kk = np.arange(128)[:, None]; qq = np.arange(128)[None, :]
    mask2 = np.concatenate([(kk <= qq), (kk >= qq)], axis=1).astype(np.float32)
    mnew = np.zeros((NST, NST), np.float32)
    for b in range(NSB):
        for t in range(TS):
            for t2 in range(TS):
                if t2 == t:
                    mnew[b * TS + t2, b * TS + t] = 3.0
                elif t2 < t:
                    mnew[b * TS + t2, b * TS + t] = 1.0
    mask1s = (np.arange(128)[:, None] >= np.arange(TS)[None, :]).astype(np.float32)
    return {"ident": ident, "selp": selp, "sels": sels, "mask2": mask2, "mnew": mnew, "mask1s": mask1s}


def _state_layout(a):
    return np.ascontiguousarray(a.reshape(16, 2, 64).transpose(1, 2, 0).reshape(128, 16))


def _pad_B(b):
    out = np.zeros((128, 16, 128), np.float32)
    for g in range(32):
        j, a = g // 2, g % 2
        out[16 * (g % 8):16 * (g % 8) + 16, j, 64 * a:64 * a + 64] = b[g].T
    return out


def _pad_C(c):
    out = np.zeros((128, 16, 128), np.float32)
    for g in range(32):
        j, a = g // 2, g % 2
        out[64 * a:64 * a + 64, j, 16 * (g % 8):16 * (g % 8) + 16] = c[g].T
    return out


_NC_CACHE = {}
_BUILD_ARGS = {}


def kernel(**inputs):
    f = lambda a: np.ascontiguousarray(np.asarray(a, dtype=np.float32))
    x_prompt = f(inputs["x_prompt"]); x_sample = f(inputs["x_sample"])
    c_prompt = f(inputs["c_prompt"]); c_sample = f(inputs["c_sample"])
    cache_k = f(inputs["cache_k_win"])[0].reshape(NCORES * NSB, WBUF, 512)
    cache_v = f(inputs["cache_v_win"])[0].reshape(NCORES * NSB, WBUF, 512)
    st_re = f(inputs["state_ssm_re"])[0]; st_im = f(inputs["state_ssm_im"])[0]
    shared = {
        "w_ada_mix": f(inputs["w_ada_mix"][0]), "b_ada_mix": f(inputs["b_ada_mix"][0]).reshape(1, -1), "g_mix": f(inputs["g_mix"][0]).reshape(1, -1),
        "w_ada_ffn": f(inputs["w_ada_ffn"][0]), "b_ada_ffn": f(inputs["b_ada_ffn"][0]).reshape(1, -1), "g_ffn": f(inputs["g_ffn"][0]).reshape(1, -1),
        "g_final": f(inputs["g_final"]).reshape(1, -1),
        "w_in": f(inputs["w_in"][0]), "w_out": f(inputs["w_out"][0]),
        "gA": f(f(inputs["g_attn_out"][0]).reshape(NH, DH).T), "gS": f(f(inputs["g_ssm_out"][0]).reshape(4, 128).T), "gA2": f(f(inputs["g_attn_out"][0]).reshape(4, 128).T),
        "w_glu": f(inputs["w_glu"][0]), "bglu": f(f(inputs["b_glu"][0]).reshape(4, 128).T),
        "lamre": _state_layout(f(inputs["ssm_lambda_re"][0])), "lamim": _state_layout(f(inputs["ssm_lambda_im"][0])),
        "logdt": _state_layout(np.repeat(f(inputs["ssm_log_dt"][0])[:, None], 64, axis=1)),
        "Bre": _pad_B(f(inputs["ssm_b_re"][0])), "Bim": _pad_B(f(inputs["ssm_b_im"][0])),
        "Cre": _pad_C(f(inputs["ssm_c_re"][0])), "Cim": _pad_C(f(inputs["ssm_c_im"][0])),
        "Dcol": f(f(inputs["ssm_d"][0]).reshape(4, 128).T),
        "w_rt": f(np.concatenate([f(inputs["w_router_coarse"][0]), f(inputs["w_router_fine"][0])], axis=1)),
        "b_rt": f(np.concatenate([f(inputs["b_router_coarse"][0]), f(inputs["b_router_fine"][0])])).reshape(1, -1),
        "w_eg": f(inputs["w_expert_gate"][0]), "w_eu": f(inputs["w_expert_up"][0]), "w_ed": f(inputs["w_expert_down"][0]),
    }
    shared.update(_consts())
    in_maps = []
    for c in range(NCORES):
        m = dict(shared)
        m["xp"] = x_prompt[c * NSEQ:(c + 1) * NSEQ]
        m["xs"] = x_sample[c * NSB:(c + 1) * NSB].reshape(NST, D)
        m["cvec"] = np.concatenate([c_prompt[c * NSEQ:(c + 1) * NSEQ], c_sample[c * NSB:(c + 1) * NSB]], axis=0)
        m["ck"] = cache_k[c * NSB:(c + 1) * NSB]; m["cv"] = cache_v[c * NSB:(c + 1) * NSB]
        lay = lambda a: np.ascontiguousarray(a[c * NSB:(c + 1) * NSB].reshape(NSB, 16, 2, 64).transpose(2, 3, 1, 0).reshape(128, 16, NSB))
        m["h0re"] = lay(st_re); m["h0im"] = lay(st_im)
        in_maps.append(m)
    if "nc" not in _NC_CACHE:
        _NC_CACHE["nc"] = build(**_BUILD_ARGS)
    nc, declared = _NC_CACHE["nc"]
    in_maps = [{k: m[k] for k in declared} for m in in_maps]
    res = run_bass_kernel_spmd(nc, in_maps, core_ids=list(range(NCORES)))
    R = res.results
    cat = lambda k: np.concatenate([r[k] for r in R], axis=0)
    B = NCORES * NSEQ
    yp = cat("y_p").reshape(B, S, D)
    ys = cat("y_s").reshape(NCORES * NSB, TS, D)
    kp = cat("k_out_p").reshape(1, B, S, NH, DH); vp = cat("v_out_p").reshape(1, B, S, NH, DH)
    ks = cat("k_out_s").reshape(1, NCORES * NSB, TS, NH, DH); vs = cat("v_out_s").reshape(1, NCORES * NSB, TS, NH, DH)
    unl = lambda a: np.ascontiguousarray(a.reshape(-1, 2, 64, 16).transpose(0, 3, 1, 2).reshape(1, -1, 32, 64))
    spr = unl(cat("ssm_p_re")); spi = unl(cat("ssm_p_im"))
    unls = lambda k: np.concatenate([r[k].reshape(2, 64, 16, NSB).transpose(3, 2, 0, 1).reshape(NSB, 32, 64) for r in R], axis=0)[None]
    ssr = np.ascontiguousarray(unls("ssm_s_re")); ssi = np.ascontiguousarray(unls("ssm_s_im"))
    return (yp, ys, kp, vp, spr, spi, ks, vs, ssr, ssi)
```
